# Optimizing a Trainium2 kernel written in Bass

```python
import math
import jax, jax.numpy as jnp
from jax import lax
import numpy as np

D_MODEL = 1024
BATCH = 8
SEQ = 2048
DEPTH = 4

GRID_W = 64
CTX_LEN = 256
Q_BLOCK = 128
HEAD_DIM = 64
ROPE_BASE = 10000.0
GQA_Q_HEADS = 8
GQA_KV_HEADS = 2
GQA_GROUP = GQA_Q_HEADS // GQA_KV_HEADS
DIFF_HEADS = 4
DIFF_V_DIM = 2 * HEAD_DIM
GQA_Q_W = GQA_Q_HEADS * HEAD_DIM
GQA_KV_W = GQA_KV_HEADS * HEAD_DIM
DIFF_QK_W = DIFF_HEADS * 2 * HEAD_DIM
DIFF_V_W = DIFF_HEADS * DIFF_V_DIM
ATTN_IN_W = GQA_Q_W + 2 * GQA_KV_W + 2 * DIFF_QK_W + DIFF_V_W
ATTN_OUT_W = GQA_Q_W + DIFF_V_W
ATTN_SPLITS = (GQA_Q_W, GQA_Q_W + GQA_KV_W, GQA_Q_W + 2 * GQA_KV_W,
               GQA_Q_W + 2 * GQA_KV_W + DIFF_QK_W, GQA_Q_W + 2 * GQA_KV_W + 2 * DIFF_QK_W)
SSM_GROUP_CH = 16
SSM_GROUPS = D_MODEL // SSM_GROUP_CH
SSM_STATE = 64
SSM_DT_MIN = 0.001
SSM_DT_MAX = 0.1
MOE_GROUPS = 4
MOE_EXPERTS_PER_GROUP = 8
MOE_EXPERTS = MOE_GROUPS * MOE_EXPERTS_PER_GROUP
MOE_TOP_K = 2
MOE_HIDDEN = D_MODEL // 4
RMS_EPS = 1e-6

kernel_name = 'hybrid_gqa_diffattn_s5_hmoe_dit'


def rms_norm(x, g):
    xf = x.astype(jnp.float32)
    y = xf * lax.rsqrt(jnp.mean(xf * xf, axis=-1, keepdims=True) + RMS_EPS)
    return (y * g.astype(jnp.float32)).astype(x.dtype)


def modulate(x, shift, scale):
    return x * (1 + scale) + shift


def axial_rope_tables(seq_len, dtype):
    n_rows = seq_len // GRID_W
    rows = jnp.repeat(jnp.arange(n_rows, dtype=jnp.float32), GRID_W)
    cols = jnp.tile(jnp.arange(GRID_W, dtype=jnp.float32), n_rows)
    half = HEAD_DIM // 2
    inv = 1.0 / (ROPE_BASE ** (jnp.arange(0, half, 2, dtype=jnp.float32) / half))
    ang_r = rows[:, None] * inv
    ang_c = cols[:, None] * inv
    ang = jnp.concatenate([ang_r, ang_r, ang_c, ang_c], axis=-1)
    return jnp.cos(ang).astype(dtype)[None, :, None, :], jnp.sin(ang).astype(dtype)[None, :, None, :]


def apply_rope(x, cos, sin):
    xs = x.reshape(x.shape[:-1] + (2, 2, HEAD_DIM // 4))
    rot = jnp.stack([-xs[..., 1, :], xs[..., 0, :]], axis=-2).reshape(x.shape)
    return x * cos + rot * sin


def sweep_query_blocks(fn, qs):
    b, lq = qs[0].shape[:2]
    nb = lq // Q_BLOCK
    blocks = tuple(jnp.moveaxis(q.reshape((b, nb, Q_BLOCK) + q.shape[2:]), 1, 0) for q in qs)
    out = lax.map(fn, blocks)
    return jnp.moveaxis(out, 0, 1).reshape((b, lq) + out.shape[3:])


def gqa_attend(q, k, v):
    scale = HEAD_DIM ** -0.5

    def blk(qs):
        (qb,) = qs
        s = jnp.einsum('bqhgd,bkhd->bhgqk', qb, k, preferred_element_type=jnp.float32) * scale
        p = jax.nn.softmax(s, axis=-1).astype(v.dtype)
        return jnp.einsum('bhgqk,bkhd->bqhgd', p, v)

    return sweep_query_blocks(blk, (q,))


def diff_attend(q1, q2, k1, k2, v, lam):
    scale = HEAD_DIM ** -0.5

    def blk(qs):
        q1b, q2b = qs
        s1 = jnp.einsum('bqhd,bkhd->bhqk', q1b, k1, preferred_element_type=jnp.float32) * scale
        s2 = jnp.einsum('bqhd,bkhd->bhqk', q2b, k2, preferred_element_type=jnp.float32) * scale
        p = jax.nn.softmax(s1, axis=-1) - lam * jax.nn.softmax(s2, axis=-1)
        return jnp.einsum('bhqk,bkhe->bqhe', p.astype(v.dtype), v)

    return sweep_query_blocks(blk, (q1, q2))


def attention_mixer(xn, cn, w_in, w_out, q_g, k_g, lq1, lk1, lq2, lk2, subln_g, lambda_init, with_ctx_out):
    bsz = xn.shape[0]

    def project(t, with_pos):
        n = t.shape[1]
        hp = t @ w_in
        qa, ka, va, qb, kb, vb = jnp.split(hp, ATTN_SPLITS, axis=-1)
        qa = rms_norm(qa.reshape(bsz, n, GQA_Q_HEADS, HEAD_DIM), q_g)
        ka = rms_norm(ka.reshape(bsz, n, GQA_KV_HEADS, HEAD_DIM), k_g)
        va = va.reshape(bsz, n, GQA_KV_HEADS, HEAD_DIM)
        qb = qb.reshape(bsz, n, 2 * DIFF_HEADS, HEAD_DIM)
        kb = kb.reshape(bsz, n, 2 * DIFF_HEADS, HEAD_DIM)
        vb = vb.reshape(bsz, n, DIFF_HEADS, DIFF_V_DIM)
        if with_pos:
            cos, sin = axial_rope_tables(n, t.dtype)
            qa, ka, qb, kb = (apply_rope(a, cos, sin) for a in (qa, ka, qb, kb))
        return qa, ka, va, qb, kb, vb

    lam = (jnp.exp(jnp.sum(lq1.astype(jnp.float32) * lk1.astype(jnp.float32)))
           - jnp.exp(jnp.sum(lq2.astype(jnp.float32) * lk2.astype(jnp.float32))) + lambda_init)

    def mix(qa, qb, ka, va, kb, vb):
        n = qa.shape[1]
        oa = gqa_attend(qa.reshape(bsz, n, GQA_KV_HEADS, GQA_GROUP, HEAD_DIM), ka, va)
        qb5 = qb.reshape(bsz, n, DIFF_HEADS, 2, HEAD_DIM)
        kb5 = kb.reshape(bsz, kb.shape[1], DIFF_HEADS, 2, HEAD_DIM)
        ob = diff_attend(qb5[..., 0, :], qb5[..., 1, :], kb5[..., 0, :], kb5[..., 1, :], vb, lam)
        ob = rms_norm(ob, subln_g) * (1.0 - lambda_init)
        merged = jnp.concatenate([oa.reshape(bsz, n, GQA_Q_W), ob.reshape(bsz, n, DIFF_V_W)], axis=-1)
        return merged @ w_out

    lqa, lka, lva, lqb, lkb, lvb = project(xn, True)
    cqa, cka, cva, cqb, ckb, cvb = project(cn, False)
    cat = lambda a, b: jnp.concatenate([a, b], axis=1)
    y_lat = mix(lqa, lqb, cat(cka, lka), cat(cva, lva), cat(ckb, lkb), cat(cvb, lvb))
    y_ctx = mix(cqa, cqb, cka, cva, ckb, cvb) if with_ctx_out else None
    return y_lat, y_ctx


def cmul(ar, ai, br, bi):
    return ar * br - ai * bi, ar * bi + ai * br


def lti_combine(e1, e2):
    a1r, a1i, b1r, b1i = e1
    a2r, a2i, b2r, b2i = e2
    ar, ai = cmul(a2r, a2i, a1r, a1i)
    br, bi = cmul(a2r, a2i, b1r, b1i)
    return ar, ai, br + b2r, bi + b2i


def diag_scan(lbar_re, lbar_im, bu_re, bu_im, h0, reverse):
    l = bu_re.shape[1]
    shape = (1, l) + lbar_re.shape
    elems = (jnp.broadcast_to(lbar_re, shape), jnp.broadcast_to(lbar_im, shape), bu_re, bu_im)
    a_re, a_im, s_re, s_im = lax.associative_scan(lti_combine, elems, reverse=reverse, axis=1)
    if h0 is not None:
        dr, di = cmul(a_re, a_im, h0[0], h0[1])
        s_re = s_re + dr
        s_im = s_im + di
    return s_re, s_im


def s5_direction(u_lat, u_ctx, a_re, a_im, log_dt, b_re, b_im, c_re, c_im, reverse, with_ctx_out):
    f32 = jnp.float32
    lre = jnp.minimum(a_re.astype(f32), -1e-4)
    lim = a_im.astype(f32)
    dt = jnp.exp(log_dt.astype(f32))[:, None]
    mag = jnp.exp(lre * dt)
    lbar_re = mag * jnp.cos(lim * dt)
    lbar_im = mag * jnp.sin(lim * dt)
    nr = lbar_re - 1.0
    ni = lbar_im
    den = lre * lre + lim * lim
    coef_re = (nr * lre + ni * lim) / den
    coef_im = (ni * lre - nr * lim) / den
    bb_re, bb_im = cmul(coef_re[..., None], coef_im[..., None], b_re.astype(f32), b_im.astype(f32))
    cr = c_re.astype(f32)
    ci = c_im.astype(f32)

    def drive(u):
        ug = u.reshape(u.shape[:2] + (SSM_GROUPS, SSM_GROUP_CH))
        return jnp.einsum('gpc,blgc->blgp', bb_re, ug), jnp.einsum('gpc,blgc->blgp', bb_im, ug)

    def readout(s_re, s_im):
        y = jnp.einsum('gcp,blgp->blgc', cr, s_re) - jnp.einsum('gcp,blgp->blgc', ci, s_im)
        return y.reshape(y.shape[:2] + (D_MODEL,))

    cs_re, cs_im = diag_scan(lbar_re, lbar_im, *drive(u_ctx), None, reverse)
    idx = 0 if reverse else cs_re.shape[1] - 1
    h0 = (cs_re[:, idx:idx + 1], cs_im[:, idx:idx + 1])
    ls_re, ls_im = diag_scan(lbar_re, lbar_im, *drive(u_lat), h0, reverse)
    y_lat = readout(ls_re, ls_im)
    y_ctx = readout(cs_re, cs_im) if with_ctx_out else None
    return y_lat, y_ctx


def s5_mixer(xn, cn, a_re, a_im, log_dt, b_re, b_im, c_re, c_im, d_skip, w_a, w_b, with_ctx_out):
    u = xn.astype(jnp.float32)
    uc = cn.astype(jnp.float32)
    yf, ycf = s5_direction(u, uc, a_re[0], a_im[0], log_dt[0], b_re[0], b_im[0], c_re[0], c_im[0], False, with_ctx_out)
    yb, ycb = s5_direction(u, uc, a_re[1], a_im[1], log_dt[1], b_re[1], b_im[1], c_re[1], c_im[1], True, with_ctx_out)
    d = d_skip.astype(jnp.float32)

    def out(y, uu):
        y = jax.nn.gelu(y + d * uu).astype(xn.dtype)
        return (y @ w_a) * jax.nn.sigmoid(y @ w_b)

    y_lat = out(yf + yb, u)
    y_ctx = out(ycf + ycb, uc) if with_ctx_out else None
    return y_lat, y_ctx


def moe_tokens(xt, gw, gb, rw, rb, w_gate, w_up, w_down):
    f32 = jnp.float32
    xf = xt.astype(f32)
    g_logits = xf @ gw.astype(f32) + gb.astype(f32)
    g_prob = jax.nn.softmax(g_logits, axis=-1)
    _, g_idx = lax.top_k(g_logits, 1)
    g_onehot = jax.nn.one_hot(g_idx[:, 0], MOE_GROUPS, dtype=f32)
    p_group = jnp.sum(g_prob * g_onehot, axis=-1, keepdims=True)
    e_logits = jnp.einsum('sd,gde->sge', xf, rw.astype(f32)) + rb.astype(f32)
    e_sel = jnp.einsum('sge,sg->se', e_logits, g_onehot)
    e_val, e_idx = lax.top_k(e_sel, MOE_TOP_K)
    w_top = jax.nn.softmax(e_val, axis=-1) * p_group
    expert = g_idx * MOE_EXPERTS_PER_GROUP + e_idx
    combine = jnp.sum(jax.nn.one_hot(expert, MOE_EXPERTS, dtype=f32) * w_top[..., None], axis=1)
    hid = jax.nn.silu(jnp.einsum('sd,edf->sef', xt, w_gate)) * jnp.einsum('sd,edf->sef', xt, w_up)
    hid = hid * combine[..., None].astype(hid.dtype)
    return jnp.einsum('sef,efd->sd', hid, w_down)


def hier_moe(x, gw, gb, rw, rb, w_gate, w_up, w_down):
    return lax.map(lambda xb: moe_tokens(xb, gw, gb, rw, rb, w_gate, w_up, w_down), x)


def setup_inputs(seed: int = 0) -> dict:
    key = jax.random.key(seed)
    ks = iter(jax.random.split(key, 48))
    f32 = jnp.float32
    D = D_MODEL
    n_attn = (DEPTH + 1) // 2
    n_ssm = DEPTH // 2

    def nrm(shape, std):
        return jax.random.normal(next(ks), shape, f32) * std

    def gain(shape):
        return 1.0 + nrm(shape, 0.02)

    return {
        'x': nrm((BATCH, SEQ, D), 1.0),
        'c': nrm((BATCH, D), 1.0),
        'ctx': nrm((BATCH, CTX_LEN, D), 1.0),
        'c_ctx': nrm((D,), 1.0),
        'mod_w': nrm((DEPTH, D, 6 * D), 0.5 * D ** -0.5),
        'mod_b': nrm((DEPTH, 6 * D), 0.02),
        'norm1_g': gain((DEPTH, D)),
        'norm2_g': gain((DEPTH, D)),
        'final_g': gain((D,)),
        'attn_w_in': nrm((n_attn, D, ATTN_IN_W), D ** -0.5),
        'attn_w_out': nrm((n_attn, ATTN_OUT_W, D), ATTN_OUT_W ** -0.5),
        'attn_q_norm_g': gain((n_attn, HEAD_DIM)),
        'attn_k_norm_g': gain((n_attn, HEAD_DIM)),
        'diff_lambda_q1': nrm((n_attn, HEAD_DIM), 0.1),
        'diff_lambda_k1': nrm((n_attn, HEAD_DIM), 0.1),
        'diff_lambda_q2': nrm((n_attn, HEAD_DIM), 0.1),
        'diff_lambda_k2': nrm((n_attn, HEAD_DIM), 0.1),
        'diff_subln_g': gain((n_attn, DIFF_V_DIM)),
        'ssm_a_re': -0.5 + nrm((n_ssm, 2, SSM_GROUPS, SSM_STATE), 0.01),
        'ssm_a_im': math.pi * jnp.arange(SSM_STATE, dtype=f32) + nrm((n_ssm, 2, SSM_GROUPS, SSM_STATE), 0.01),
        'ssm_log_dt': jax.random.uniform(next(ks), (n_ssm, 2, SSM_GROUPS), f32,
                                         math.log(SSM_DT_MIN), math.log(SSM_DT_MAX)),
        'ssm_b_re': nrm((n_ssm, 2, SSM_GROUPS, SSM_STATE, SSM_GROUP_CH), (2 * SSM_GROUP_CH) ** -0.5),
        'ssm_b_im': nrm((n_ssm, 2, SSM_GROUPS, SSM_STATE, SSM_GROUP_CH), (2 * SSM_GROUP_CH) ** -0.5),
        'ssm_c_re': nrm((n_ssm, 2, SSM_GROUPS, SSM_GROUP_CH, SSM_STATE), (2 * SSM_STATE) ** -0.5),
        'ssm_c_im': nrm((n_ssm, 2, SSM_GROUPS, SSM_GROUP_CH, SSM_STATE), (2 * SSM_STATE) ** -0.5),
        'ssm_d': nrm((n_ssm, D), 1.0),
        'ssm_glu_w_a': nrm((n_ssm, D, D), D ** -0.5),
        'ssm_glu_w_b': nrm((n_ssm, D, D), D ** -0.5),
        'moe_group_w': nrm((DEPTH, D, MOE_GROUPS), D ** -0.5),
        'moe_group_b': nrm((DEPTH, MOE_GROUPS), 0.01),
        'moe_router_w': nrm((DEPTH, MOE_GROUPS, D, MOE_EXPERTS_PER_GROUP), D ** -0.5),
        'moe_router_b': nrm((DEPTH, MOE_GROUPS, MOE_EXPERTS_PER_GROUP), 0.01),
        'moe_w_gate': nrm((DEPTH, MOE_EXPERTS, D, MOE_HIDDEN), D ** -0.5),
        'moe_w_up': nrm((DEPTH, MOE_EXPERTS, D, MOE_HIDDEN), D ** -0.5),
        'moe_w_down': nrm((DEPTH, MOE_EXPERTS, MOE_HIDDEN, D), MOE_HIDDEN ** -0.5),
    }


def reference(x, c, ctx, c_ctx, mod_w, mod_b, norm1_g, norm2_g, final_g,
              attn_w_in, attn_w_out, attn_q_norm_g, attn_k_norm_g,
              diff_lambda_q1, diff_lambda_k1, diff_lambda_q2, diff_lambda_k2, diff_subln_g,
              ssm_a_re, ssm_a_im, ssm_log_dt, ssm_b_re, ssm_b_im, ssm_c_re, ssm_c_im, ssm_d,
              ssm_glu_w_a, ssm_glu_w_b,
              moe_group_w, moe_group_b, moe_router_w, moe_router_b, moe_w_gate, moe_w_up, moe_w_down):
    h, hc = x, ctx
    for layer in range(DEPTH):
        last = layer == DEPTH - 1
        m = [t[:, None, :] for t in jnp.split(jax.nn.silu(c) @ mod_w[layer] + mod_b[layer], 6, axis=-1)]
        mc = [t[None, None, :] for t in jnp.split(jax.nn.silu(c_ctx) @ mod_w[layer] + mod_b[layer], 6, axis=-1)]
        xn = modulate(rms_norm(h, norm1_g[layer]), m[0], m[1])
        cn = modulate(rms_norm(hc, norm1_g[layer]), mc[0], mc[1])
        i = layer // 2
        if layer % 2 == 0:
            lambda_init = 0.8 - 0.6 * math.exp(-0.3 * layer)
            y, yc = attention_mixer(xn, cn, attn_w_in[i], attn_w_out[i], attn_q_norm_g[i], attn_k_norm_g[i],
                                    diff_lambda_q1[i], diff_lambda_k1[i], diff_lambda_q2[i], diff_lambda_k2[i],
                                    diff_subln_g[i], lambda_init, not last)
        else:
            y, yc = s5_mixer(xn, cn, ssm_a_re[i], ssm_a_im[i], ssm_log_dt[i], ssm_b_re[i], ssm_b_im[i],
                             ssm_c_re[i], ssm_c_im[i], ssm_d[i], ssm_glu_w_a[i], ssm_glu_w_b[i], not last)
        moe_p = (moe_group_w[layer], moe_group_b[layer], moe_router_w[layer], moe_router_b[layer],
                 moe_w_gate[layer], moe_w_up[layer], moe_w_down[layer])
        h = h + m[2] * y
        h = h + m[5] * hier_moe(modulate(rms_norm(h, norm2_g[layer]), m[3], m[4]), *moe_p)
        if not last:
            hc = hc + mc[2] * yc
            hc = hc + mc[5] * hier_moe(modulate(rms_norm(hc, norm2_g[layer]), mc[3], mc[4]), *moe_p)
    return rms_norm(h, final_g)
```

```python
import contextlib
import math
import numpy as np
import concourse.bass as bass
import concourse.mybir as mybir
from concourse.bass_utils import run_bass_kernel_spmd

F32 = mybir.dt.float32
BF16 = mybir.dt.bfloat16
ALU = mybir.AluOpType
AF = mybir.ActivationFunctionType
AX = mybir.AxisListType

EPOCH = 30000
NDMASEM = 24
DEPTH = 4
NT = 2304
NCX = 256
EPS = 1e-6
BLKS = [(0, 256)] + [(256 + 512 * i, 512) for i in range(4)]
TB = 32
NLAYERS = 4


class Prog:
    def __init__(self, nc, stack):
        self.nc = nc
        self.stack = stack
        self.engs = {'pe': nc.tensor, 'act': nc.scalar, 'dve': nc.vector,
                     'pool': nc.gpsimd, 'sp': nc.sync}
        self.streams = {e: [] for e in self.engs}
        self.cnt = {e: 0 for e in self.engs}
        self.esems = {e: [] for e in self.engs}
        self.known = {e: {} for e in self.engs}
        self.last_w = {}
        self.readers = {}
        self.dsems = []
        self.dcnt = []
        self.dnext = [0, 0]
        self.nsem = 0
        self.pesem = set()
        self.psi = 0

    def _newsem(self, name):
        self.nsem += 1
        return self.stack.enter_context(self.nc.semaphore(name))

    def _esem(self, e):
        k = self.cnt[e] // EPOCH
        while len(self.esems[e]) <= k:
            s = self._newsem(f"s_{e}_{len(self.esems[e])}")
            self.esems[e].append(s)
            if e == 'pe':
                self.pesem.add(id(s))
        return self.esems[e][k], self.cnt[e] % EPOCH + 1

    def _deps(self, e, reads, writes):
        deps = {}

        def add(t):
            if t is None:
                return
            s, v = t
            if e == 'pe' and id(s) in self.pesem:
                return
            if deps.get(id(s), (s, 0))[1] < v:
                deps[id(s)] = (s, v)
        for k in reads:
            add(self.last_w.get(k))
        for k in writes:
            add(self.last_w.get(k))
            for t in self.readers.get(k, ()):
                add(t)
        out = []
        kn = self.known[e]
        for sid, (s, v) in deps.items():
            if kn.get(sid, 0) < v:
                kn[sid] = v
                out.append((s, v))
        return out

    def _mark(self, reads, writes, tok):
        for k in reads:
            self.readers.setdefault(k, []).append(tok)
        for k in writes:
            self.last_w[k] = tok
            self.readers[k] = []

    def op(self, e, fn, reads=(), writes=()):
        waits = self._deps(e, reads, writes)
        sem, val = self._esem(e)
        self.cnt[e] += 1

        def run(eng, waits=waits, fn=fn, sem=sem):
            for s, v in waits:
                eng.wait_ge(s, v)
            fn(eng).then_inc(sem, 1)
        self.streams[e].append(run)
        self._mark(reads, writes, (sem, val))

    def dma(self, e, out, in_, reads=(), writes=()):
        lo, n = (0, NDMASEM - 8) if e != 'pool' else (NDMASEM - 8, 8)
        while len(self.dsems) < NDMASEM:
            self.dsems.append(self._newsem(f"s_dma_{len(self.dsems)}"))
            self.dcnt.append(0)
        i = lo + self.dnext[e != 'pool']
        self.dnext[e != 'pool'] = (self.dnext[e != 'pool'] + 1) % n
        if self.dcnt[i] * 16 >= 30000:
            self.dsems[i] = self._newsem(f"s_dma_{i}_x{self.nsem}")
            self.dcnt[i] = 0
        sem = self.dsems[i]
        waits = self._deps(e, reads, writes)
        prev = self.dcnt[i] * 16
        kn = self.known[e]
        if prev and kn.get(id(sem), 0) < prev:
            kn[id(sem)] = prev
            waits.append((sem, prev))
        self.dcnt[i] += 1
        val = self.dcnt[i] * 16

        def run(eng, waits=waits, sem=sem):
            for s, v in waits:
                eng.wait_ge(s, v)
            eng.dma_start(out=out, in_=in_).then_inc(sem, 16)
        self.streams[e].append(run)
        self._mark(reads, writes, (sem, val))

    def barrier(self):
        allw = []
        for i, sm in enumerate(self.dsems):
            if self.dcnt[i]:
                allw.append((sm, self.dcnt[i] * 16))
        for en in self.engs:
            if self.cnt[en]:
                k = (self.cnt[en] - 1) // EPOCH
                allw.append((self.esems[en][k], (self.cnt[en] - 1) % EPOCH + 1))
        for e in self.engs:
            kn = self.known[e]
            waits = []
            for sm, v in allw:
                if kn.get(id(sm), 0) < v:
                    kn[id(sm)] = v
                    waits.append((sm, v))

            def run(eng, waits=waits):
                for sm, v in waits:
                    eng.wait_ge(sm, v)
            self.streams[e].append(run)
        self.last_w.clear()
        self.readers.clear()

    def finish(self, e='sp'):
        waits = []
        for i, s in enumerate(self.dsems):
            if self.dcnt[i]:
                waits.append((s, self.dcnt[i] * 16))
        for en in self.engs:
            if self.cnt[en] and en != e:
                k = (self.cnt[en] - 1) // EPOCH
                waits.append((self.esems[en][k], (self.cnt[en] - 1) % EPOCH + 1))

        def run(eng):
            for s, v in waits:
                eng.wait_ge(s, v)
        self.streams[e].append(run)

    def emit(self):
        with self.nc.Block() as block:
            @block.sync
            def _(eng):
                for c in self.streams['sp']:
                    c(eng)

            @block.tensor
            def _(eng):
                for c in self.streams['pe']:
                    c(eng)

            @block.scalar
            def _(eng):
                for c in self.streams['act']:
                    c(eng)

            @block.vector
            def _(eng):
                for c in self.streams['dve']:
                    c(eng)

            @block.gpsimd
            def _(eng):
                for c in self.streams['pool']:
                    c(eng)

    def mm(self, out, pairs, reads, writes):
        def fn(e, out=out, pairs=pairs):
            n = len(pairs)
            ins = None
            for i, (l, r) in enumerate(pairs):
                ins = e.matmul(out, l, r, start=(i == 0), stop=(i == n - 1))
            return ins
        self.op('pe', fn, reads, writes)

    def mm1(self, out, l, r, start, stop, reads, writes):
        self.op('pe', lambda e: e.matmul(out, l, r, start=start, stop=stop), reads, writes)

    def tr(self, out, in_, ident, reads, writes):
        self.op('pe', lambda e: e.transpose(out, in_, ident), reads, writes)

    def act(self, out, in_, func, reads, writes, **kw):
        self.op('act', lambda e: e.activation(out, in_, func, **kw), reads, writes)

    def tt(self, eng, out, a, b, op, reads, writes):
        self.op(eng, lambda e: e.tensor_tensor(out, a, b, op), reads, writes)

    def ts(self, eng, out, a, s1, s2, op0, op1, reads, writes):
        self.op(eng, lambda e: e.tensor_scalar(out, a, s1, s2, op0, op1), reads, writes)

    def ts1(self, eng, out, a, s, op, reads, writes):
        self.op(eng, lambda e: e.tensor_single_scalar(out, a, s, op), reads, writes)

    def stt(self, eng, out, a, s, b, op0, op1, reads, writes):
        self.op(eng, lambda e: e.scalar_tensor_tensor(out, a, s, b, op0, op1), reads, writes)

    def cp(self, eng, out, a, reads, writes):
        self.op(eng, lambda e: e.tensor_copy(out, a), reads, writes)

    def memset(self, eng, ap, v, writes):
        self.op(eng, lambda e: e.memset(ap, v), (), writes)

    def recip(self, out, a, reads, writes):
        self.op('dve', lambda e: e.reciprocal(out, a), reads, writes)


class Rot:
    def __init__(self, tiles, name):
        self.tiles = tiles
        self.name = name
        self.i = 0

    def next(self):
        i = self.i
        self.i = (i + 1) % len(self.tiles)
        return self.tiles[i], (self.name, i)


ARENA = 39000


class Carver:
    def __init__(self, arena):
        self.arena = arena
        self.o = 0
        self.bf = arena[:].bitcast(BF16)

    def f(self, n):
        a = self.o
        self.o += n
        assert self.o <= ARENA, self.o
        return self.arena[:, a:a + n]

    def b(self, n):
        n2 = (n + 1) // 2
        a = self.o
        self.o += n2
        assert self.o <= ARENA, self.o
        return self.bf[:, 2 * a:2 * a + n]


class Rot:
    def __init__(self, tiles, name):
        self.tiles = tiles
        self.name = name
        self.i = 0

    def next(self):
        i = self.i
        self.i = (i + 1) % len(self.tiles)
        return self.tiles[i], (self.name, i)


def build():
    nc = bass.Bass("TRN2", target_bir_lowering=False)

    def din(name, shape):
        return nc.dram_tensor(name, list(shape), F32, kind="ExternalInput").ap()

    xT = din("xT", [1024, NT])
    cT = din("cT", [128, 8, 2])
    mod_w = din("mod_w", [4, 1024, 6144])
    mod_bT = din("mod_bT", [4, 128, 48])
    n1g = din("n1g", [4, 128, 8])
    n2g = din("n2g", [4, 128, 8])
    fgT = din("fgT", [128, 8])
    w_in = din("w_in", [2, 19, 1024, 128])
    w_out = din("w_out", [2, 1024, 1024])
    qg = din("qg", [2, 128, 1])
    kg = din("kg", [2, 128, 1])
    slg = din("slg", [2, 128, 1])
    lamv = din("lamv", [2, 128, 4, 64])
    cosT = din("cosT", [128, NT])
    sinT = din("sinT", [128, NT])
    rmat = din("rmat", [128, 128])
    ident = din("ident", [128, 128])
    bones = din("bones", [128, 128])
    s_are = din("s_are", [2, 2, 128, 32])
    s_aim = din("s_aim", [2, 2, 128, 32])
    s_ldt = din("s_ldt", [2, 2, 128, 32])
    s_bre = din("s_bre", [2, 2, 128, 32, 16])
    s_bim = din("s_bim", [2, 2, 128, 32, 16])
    s_cre = din("s_cre", [2, 2, 128, 32, 16])
    s_cim = din("s_cim", [2, 2, 128, 32, 16])
    s_d = din("s_d", [2, 128, 8])
    pmask = din("pmask", [128, 8])
    glu_a = din("glu_a", [2, 1024, 1024])
    glu_b = din("glu_b", [2, 1024, 1024])
    wr = din("wr", [4, 1024, 36])
    wrb = din("wrb", [4, 128, 36])
    w_gate = din("w_gate", [4, 32, 1024, 256])
    w_up = din("w_up", [4, 32, 1024, 256])
    w_down = din("w_down", [4, 32, 256, 1024])
    outT = nc.dram_tensor("outT", [1024, 2048], F32, kind="ExternalOutput").ap()
    hT = nc.dram_tensor("hT_scr", [1024, NT], F32, kind="Internal").ap()
    yscr = [nc.dram_tensor(f"y_scr{d}", [1024, NT], F32, kind="Internal").ap() for d in range(2)]

    hT3 = hT.rearrange("(k p) t -> p k t", p=128)
    xT3 = xT.rearrange("(k p) t -> p k t", p=128)
    outT3 = outT.rearrange("(k p) t -> p k t", p=128)
    y3 = [y.rearrange("(k p) t -> p k t", p=128) for y in yscr]

    with contextlib.ExitStack() as st:
        P = Prog(nc, st)

        def sb(name, shape, dt=F32):
            return st.enter_context(nc.sbuf_tensor(name, list(shape), dt))

        ps_tiles = [st.enter_context(nc.psum_tensor(f"ps{i}", [128, 512], F32)) for i in range(8)]

        def ps_get():
            i = P.psi
            P.psi = (i + 1) % 8
            return ps_tiles[i], ('ps', i)

        arena = sb("arena", [128, ARENA])
        xn = sb("xn", [128, 8, NT], BF16)
        ones_b = sb("ones_b", [128, 128], BF16)
        ona_b = sb("ona_b", [128, 128], BF16)
        onb_b = sb("onb_b", [128, 128], BF16)
        ones_f = sb("ones_f", [128, 128])
        ident_s = sb("ident_s", [128, 128])
        bones_s = sb("bones_s", [128, 128])
        rmat_s = sb("rmat_s", [128, 128])
        pm = sb("pm", [128, 8])
        cs = sb("cs", [128, 8, 2])
        fg_s = sb("fg_s", [128, 8])
        modT = sb("modT", [128, 48, 2])
        mbias = sb("mbias", [128, 48])
        gsc = sb("gsc", [128, 2, 8, 2])
        g12 = sb("g12", [128, 2, 8])
        apar = sb("apar", [128, 8])
        lam_s = sb("lam_s", [128, 4, 64])
        wrs = sb("wrs", [128, 8, 36])
        wrb_s = sb("wrb_s", [128, 36])
        dsk = sb("dsk", [128, 8])

        P.memset('pool', ones_b[:], 1.0, ['ones_b'])
        P.memset('pool', ones_f[:], 1.0, ['ones_f'])
        P.memset('pool', ona_b[:], 0.0, ['ona_b'])
        P.memset('pool', ona_b[:, 0:64], 1.0, ['ona_b'])
        P.memset('pool', onb_b[:], 0.0, ['onb_b'])
        P.memset('pool', onb_b[:, 64:128], 1.0, ['onb_b'])
        P.dma('sp', ident_s[:], ident, writes=['ident'])
        P.dma('sp', bones_s[:], bones, writes=['bones'])
        P.dma('sp', rmat_s[:], rmat, writes=['rmat'])
        P.dma('sp', pm[:], pmask, writes=['pm'])
        P.dma('sp', cs[:], cT, writes=['cs'])
        P.dma('sp', fg_s[:], fgT, writes=['fg'])
        P.act(cs[:], cs[:], AF.Silu, ['cs'], ['cs'])

        def mkrot(C, name, n, nel, shape_fn=None, bf=False):
            tiles = []
            for i in range(n):
                t = C.b(nel) if bf else C.f(nel)
                tiles.append(shape_fn(t) if shape_fn else t)
            return Rot(tiles, name)

        k8 = lambda t: t.rearrange("p (k t) -> p k t", k=8)

        C = Carver(arena)
        hb_rot = mkrot(C, "hb", 2, 4096, k8)
        for (t0, n) in BLKS:
            hb, hk = hb_rot.next()
            P.dma('sp', hb[:, :, 0:n], xT3[:, :, t0:t0 + n], writes=[hk])
            P.dma('sp', hT3[:, :, t0:t0 + n], hb[:, :, 0:n], reads=[hk], writes=[('h', t0)])
        P.barrier()

        def phase_mod(l):
            C = Carver(arena)
            mw_rot = mkrot(C, "mw", 2, 6144, lambda t: t.rearrange("p (k f) -> p k f", k=8))
            P.dma('sp', mbias[:], mod_bT[l], writes=['mbias'])
            P.dma('sp', g12[:, 0, :], n1g[l], writes=['g12'])
            P.dma('sp', g12[:, 1, :], n2g[l], writes=['g12'])
            pst, pk = ps_get()
            for fgp in range(8):
                mw, mk = mw_rot.next()
                P.dma('sp' if fgp % 2 == 0 else 'act', mw, mod_w[l][:, fgp * 768:(fgp + 1) * 768].rearrange("(k p) f -> p k f", p=128), writes=[mk])
                for fc in range(6):
                    f = fgp * 6 + fc
                    P.mm(pst[:, 2 * f:2 * f + 2],
                         [(mw[:, k, fc * 128:(fc + 1) * 128], cs[:, k, :]) for k in range(8)],
                         [mk, 'cs'], [pk])
            for j in range(2):
                P.tt('dve', modT[:, :, j], pst[:, 0:96].rearrange("p (f j) -> p f j", j=2)[:, :, j], mbias[:],
                     ALU.add, [pk, 'mbias'], ['modT'])
            for ni, base in ((0, 8), (1, 32)):
                for j in range(2):
                    P.ts1('dve', gsc[:, ni, :, j], modT[:, base:base + 8, j], 1.0, ALU.add, ['modT'], ['gsc'])
                    P.tt('dve', gsc[:, ni, :, j], gsc[:, ni, :, j], g12[:, ni, :], ALU.mult, ['gsc', 'g12'], ['gsc'])
            P.barrier()

        def do_norm(C, ni, shift_base, router=None, nbuf=2):
            hb_rot = mkrot(C, "nhb", nbuf, 4096, k8)
            sq_rot = mkrot(C, "nsq", nbuf, 4096, k8, bf=True)
            rs_rot = mkrot(C, "nrs", 2, 512)
            for bi, (t0, n) in enumerate(BLKS):
                j = 1 if bi == 0 else 0
                hb, hk = hb_rot.next()
                P.dma('sp', hb[:, :, 0:n], hT3[:, :, t0:t0 + n], reads=[('h', t0)], writes=[hk])
                sq, sk = sq_rot.next()
                P.act(sq[:, :, 0:n], hb[:, :, 0:n], AF.Square, [hk], [sk])
                pst, pk = ps_get()
                P.mm(pst[:, 0:n], [(ones_b[:], sq[:, k, 0:n]) for k in range(8)], ['ones_b', sk], [pk])
                rs, rk = rs_rot.next()
                P.act(rs[:, 0:n], pst[:, 0:n], AF.Sqrt, [pk], [rk], bias=EPS, scale=1.0 / 1024)
                P.recip(rs[:, 0:n], rs[:, 0:n], [rk], [rk])
                for k in range(8):
                    P.stt('dve', hb[:, k, 0:n], hb[:, k, 0:n], gsc[:, ni, k, j:j + 1], rs[:, 0:n],
                          ALU.mult, ALU.mult, [hk, 'gsc', rk], [hk])
                    if router is None:
                        P.act(xn[:, k, t0:t0 + n], hb[:, k, 0:n], AF.Identity, [hk, 'modT'], [('xn', t0)],
                              bias=modT[:, shift_base + k, j:j + 1], scale=1.0)
                    else:
                        P.act(hb[:, k, 0:n], hb[:, k, 0:n], AF.Identity, [hk, 'modT'], [hk],
                              bias=modT[:, shift_base + k, j:j + 1], scale=1.0)
                if router is not None:
                    P.cp('pool', xn[:, :, t0:t0 + n], hb[:, :, 0:n], [hk], [('xn', t0)])
                    router(hb, hk, t0, n)

        def add_residual(hb_rot, t0, n, j, gate_base, get_ps):
            hb, hk = hb_rot.next()
            P.dma('sp', hb[:, :, 0:n], hT3[:, :, t0:t0 + n], reads=[('h', t0)], writes=[hk])
            for oc in range(8):
                src, srck = get_ps(oc)
                P.stt('dve', hb[:, oc, 0:n], src, modT[:, gate_base + oc, j:j + 1], hb[:, oc, 0:n],
                      ALU.mult, ALU.add, [hk, 'modT'] + srck, [hk])
            P.dma('sp', hT3[:, :, t0:t0 + n], hb[:, :, 0:n], reads=[hk], writes=[('h', t0)])

        xn_keys = [('xn', t0) for (t0, n) in BLKS]

        def phase_norm1():
            C = Carver(arena)
            do_norm(C, 0, 0)
            P.barrier()

        def phase_moe(l):
            C = Carver(arena)
            acc = k8(C.f(8 * NT))
            combT = C.f(NT)[0:32, :]
            rt_rot = mkrot(C, "rt", 2, 256)
            wg_rot = mkrot(C, "wg", 2, 2048, lambda t: t.rearrange("p (k f) -> p k f", k=8), bf=True)
            wu_rot = mkrot(C, "wu", 2, 2048, lambda t: t.rearrange("p (k f) -> p k f", k=8), bf=True)
            wd_rot = mkrot(C, "wd", 2, 2048, lambda t: t.rearrange("p (k f) -> p k f", k=2), bf=True)
            hid_rot = mkrot(C, "hid", 2, 1024, lambda t: t.rearrange("p (k f) -> p k f", k=2), bf=True)
            tmp_rot = mkrot(C, "mtmp", 3, 512)
            cme_rot = mkrot(C, "cme", 2, 512)
            P.dma('sp', wrs[:], wr[l].rearrange("(k p) f -> p k f", p=128), writes=['wrs'])
            P.dma('sp', wrb_s[:], wrb[l], writes=['wrb'])

            def router(x32, xk, t0, n):
                for tt_ in range(n // 128):
                    c0 = tt_ * 128
                    pst, pk = ps_get()
                    P.mm(pst[:, 0:36], [(x32[:, k, c0:c0 + 128], wrs[:, k, :]) for k in range(8)],
                         [xk, 'wrs'], [pk])
                    r, rk = rt_rot.next()
                    R = [rk]
                    lg = r[:, 0:36]
                    P.tt('dve', lg, pst[:, 0:36], wrb_s[:], ALU.add, [pk, 'wrb'], R)
                    gl = r[:, 0:4]
                    el = r[:, 4:36].rearrange("p (g e) -> p g e", g=4)
                    gmax = r[:, 40:41]
                    P.op('dve', lambda e, o=gmax, i=gl: e.reduce_max(o, i, AX.X), R, R)
                    goh = r[:, 44:48]
                    P.ts1('dve', goh, gl, gmax, ALU.is_ge, R, R)
                    gex = r[:, 48:52]
                    ngm = r[:, 41:42]
                    P.ts1('dve', ngm, gmax, -1.0, ALU.mult, R, R)
                    gsum = r[:, 42:43]
                    P.act(gex, gl, AF.Exp, R, R, bias=ngm, scale=1.0)
                    P.op('dve', lambda e, o=gsum, i=gex: e.reduce_sum(o, i, AX.X), R, R)
                    pgrp = r[:, 43:44]
                    P.recip(pgrp, gsum, R, R)
                    em = r[:, 64:96].rearrange("p (g e) -> p g e", g=4)
                    P.tt('dve', em, el, goh.unsqueeze(2).to_broadcast([128, 4, 8]), ALU.mult, R, R)
                    esel = r[:, 96:104]
                    P.op('dve', lambda e, o=esel, i=r[:, 64:96].rearrange("p (g e) -> p e g", g=4):
                         e.reduce_sum(o, i, AX.X), R, R)
                    m1 = r[:, 104:105]
                    P.op('dve', lambda e, o=m1, i=esel: e.reduce_max(o, i, AX.X), R, R)
                    mk1 = r[:, 112:120]
                    P.ts1('dve', mk1, esel, m1, ALU.is_ge, R, R)
                    es2 = r[:, 120:128]
                    P.stt('dve', es2, mk1, -1e30, esel, ALU.mult, ALU.add, R, R)
                    m2 = r[:, 105:106]
                    P.op('dve', lambda e, o=m2, i=es2: e.reduce_max(o, i, AX.X), R, R)
                    mk2 = r[:, 128:136]
                    P.ts1('dve', mk2, es2, m2, ALU.is_ge, R, R)
                    dm = r[:, 106:107]
                    P.tt('dve', dm, m1, m2, ALU.subtract, R, R)
                    P.act(dm, dm, AF.Exp, R, R)
                    P.ts1('dve', dm, dm, 1.0, ALU.add, R, R)
                    w2 = r[:, 107:108]
                    P.recip(w2, dm, R, R)
                    w1 = r[:, 108:109]
                    P.ts('dve', w1, w2, -1.0, 1.0, ALU.mult, ALU.add, R, R)
                    P.tt('dve', w1, w1, pgrp, ALU.mult, R, R)
                    P.tt('dve', w2, w2, pgrp, ALU.mult, R, R)
                    cl = r[:, 136:144]
                    P.ts1('dve', cl, mk1, w1, ALU.mult, R, R)
                    P.stt('dve', cl, mk2, w2, cl, ALU.mult, ALU.add, R, R)
                    comb = r[:, 160:192].rearrange("p (g e) -> p g e", g=4)
                    P.tt('dve', comb, goh.unsqueeze(2).to_broadcast([128, 4, 8]),
                         cl.unsqueeze(1).to_broadcast([128, 4, 8]), ALU.mult, R, R)
                    pt, ptk = ps_get()
                    P.tr(pt[0:32, 0:128], r[:, 160:192], ident_s[:], R + ['ident'], [ptk])
                    P.cp('dve', combT[:, t0 + c0:t0 + c0 + 128], pt[0:32, 0:128], [ptk], [('combT', t0)])

            C2 = Carver(arena)
            C2.o = C.o
            do_norm(C2, 1, 24, router=router, nbuf=1)
            hb_rot = Rot([k8(arena[:, C.o:C.o + 4096])], "nhb")

            def load(e):
                wg, wgk = wg_rot.next()
                wu, wuk = wu_rot.next()
                wd, wdk = wd_rot.next()
                P.dma('pool', wg, w_gate[l, e].rearrange("(p k) f -> p k f", k=8), writes=[wgk])
                P.dma('pool', wu, w_up[l, e].rearrange("(p k) f -> p k f", k=8), writes=[wuk])
                P.dma('pool', wd, w_down[l, e].rearrange("(k p) f -> p k f", p=128), writes=[wdk])
                return (wg, wgk, wu, wuk, wd, wdk)
            def front(e, bi, wts):
                wg, wgk, wu, wuk, wd, wdk = wts
                t0, n = BLKS[bi]
                pcb, pcbk = ps_get()
                P.mm(pcb[:, 0:n], [(ident_s[0:32, e:e + 1].to_broadcast([32, 128]), combT[:, t0:t0 + n])],
                     ['ident', ('combT', t0)], [pcbk])
                cbs, cbk = tmp_rot.next()
                P.act(cbs[:, 0:n], pcb[:, 0:n], AF.Copy, [pcbk], [cbk])
                hid, hidk = hid_rot.next()
                for hc in range(2):
                    pg, pgk = ps_get()
                    pu, puk = ps_get()
                    P.mm(pg[:, 0:n], [(wg[:, k, hc * 128:(hc + 1) * 128], xn[:, k, t0:t0 + n]) for k in range(8)],
                         [wgk, ('xn', t0)], [pgk])
                    P.mm(pu[:, 0:n], [(wu[:, k, hc * 128:(hc + 1) * 128], xn[:, k, t0:t0 + n]) for k in range(8)],
                         [wuk, ('xn', t0)], [puk])
                    sg, sgk = tmp_rot.next()
                    P.act(sg[:, 0:n], pg[:, 0:n], AF.Silu, [pgk], [sgk])
                    P.tt('dve', sg[:, 0:n], pu[:, 0:n], sg[:, 0:n], ALU.mult, [puk, sgk], [sgk])
                    P.tt('pool', hid[:, hc, 0:n], sg[:, 0:n], cbs[:, 0:n], ALU.mult, [sgk, cbk], [hidk])
                return (e, bi, hid, hidk, wd, wdk)

            def back(e, bi, hid, hidk, wd, wdk):
                t0, n = BLKS[bi]
                for dc in range(8):
                    po, pok = ps_get()
                    P.mm(po[:, 0:n], [(wd[:, hc, dc * 128:(dc + 1) * 128], hid[:, hc, 0:n]) for hc in range(2)],
                         [wdk, hidk], [pok])
                    if e == 0:
                        P.cp('dve', acc[:, dc, t0:t0 + n], po[:, 0:n], [pok], [('acc', t0)])
                    else:
                        P.tt('dve', acc[:, dc, t0:t0 + n], po[:, 0:n], acc[:, dc, t0:t0 + n], ALU.add,
                             [pok, ('acc', t0)], [('acc', t0)])

            W = {0: load(0), 1: load(1)}
            pend = None
            for e in range(32):
                for bi in range(len(BLKS)):
                    cur = front(e, bi, W[e])
                    if pend is not None:
                        back(*pend)
                    if bi == 0 and e >= 1 and e + 1 < 32:
                        W[e + 1] = load(e + 1)
                    pend = cur
            back(*pend)
            for bi, (t0, n) in enumerate(BLKS):
                j = 1 if bi == 0 else 0
                add_residual(hb_rot, t0, n, j, 40, lambda oc, t0=t0, n=n: (acc[:, oc, t0:t0 + n], [('acc', t0)]))
            P.barrier()

        def phase_attn(l):
            ia = l // 2
            lambda_init = 0.8 - 0.6 * math.exp(-0.3 * l)
            C = Carver(arena)
            merged = k8(C.b(8 * NT))
            qT = C.b(NT)
            kT = C.b(NT)
            VA = C.b(NT).rearrange("p (t c) -> p t c", c=128)
            VB = C.b(NT).rearrange("p (t c) -> p t c", c=128)
            wo = k8(C.b(8192))
            w3 = lambda t: t.rearrange("p (k f) -> p k f", k=8)
            wq_rot = mkrot(C, "wq", 2, 1024, w3, bf=True)
            wk_rot = mkrot(C, "wk", 2, 1024, w3, bf=True)
            wv_rot = mkrot(C, "wv", 2, 1024, w3, bf=True)
            tmp_rot = mkrot(C, "atmp", 4, 512)
            tmpb_rot = mkrot(C, "atmpb", 4, 512, bf=True)
            rs_rot = mkrot(C, "ars", 2, 512)
            cs_rot = mkrot(C, "acs", 2, 1024)
            hb_rot = mkrot(C, "ahb", 2, 4096, k8)
            import os as _os
            KA = int(_os.environ.get('KA', '7'))
            P.dma('sp', apar[:, 0:1], qg[ia], writes=['apar'])
            P.dma('sp', apar[:, 1:2], kg[ia], writes=['apar'])
            P.dma('sp', apar[:, 2:3], slg[ia], writes=['apar'])
            P.dma('sp', lam_s[:], lamv[ia], writes=['lam_s'])
            A = ['apar']
            if KA & 1:
              P.ts1('dve', apar[:, 2:3], apar[:, 2:3], 1.0 - lambda_init, ALU.mult, A, A)
            if KA & 1:
              P.tt('dve', lam_s[:, 0, :], lam_s[:, 0, :], lam_s[:, 1, :], ALU.mult, ['lam_s'], ['lam_s'])
            if KA & 1:
              P.tt('dve', lam_s[:, 2, :], lam_s[:, 2, :], lam_s[:, 3, :], ALU.mult, ['lam_s'], ['lam_s'])
            if KA & 1:
              P.op('dve', lambda e: e.reduce_sum(apar[:, 4:5], lam_s[:, 0, :], AX.X), ['lam_s'] + A, A)
            if KA & 1:
              P.op('dve', lambda e: e.reduce_sum(apar[:, 5:6], lam_s[:, 2, :], AX.X), ['lam_s'] + A, A)
            if KA & 1:
              P.act(apar[:, 4:6], apar[:, 4:6], AF.Exp, A, A)
            if KA & 1:
              P.tt('dve', apar[:, 3:4], apar[:, 5:6], apar[:, 4:5], ALU.subtract, A, A)
            if KA & 1:
              P.ts1('dve', apar[:, 3:4], apar[:, 3:4], -lambda_init, ALU.add, A, A)
            if KA & 2:
              P.dma('pool', wo, w_out[ia].rearrange("(k p) f -> p k f", p=128), writes=['wo'])

            def qk_post(pst, pk, is_gqa, gcol, dst, dk, t0, n):
                csb, csk = cs_rot.next()
                P.dma('sp', csb[:, 0:n], cosT[:, t0:t0 + n], writes=[csk])
                P.dma('sp', csb[:, 512:512 + n], sinT[:, t0:t0 + n], writes=[csk])
                if is_gqa:
                    sq, sk = tmp_rot.next()
                    P.act(sq[:, 0:n], pst[:, 0:n], AF.Square, [pk], [sk])
                    p2, p2k = ps_get()
                    P.mm(p2[:, 0:n], [(bones_s[:], sq[:, 0:n])], ['bones', sk], [p2k])
                    rs, rk = rs_rot.next()
                    P.act(rs[:, 0:n], p2[:, 0:n], AF.Sqrt, [p2k], [rk], bias=EPS, scale=1.0 / 64)
                    P.recip(rs[:, 0:n], rs[:, 0:n], [rk], [rk])
                    xs, xk = tmp_rot.next()
                    P.stt('dve', xs[:, 0:n], pst[:, 0:n], apar[:, gcol:gcol + 1], rs[:, 0:n], ALU.mult, ALU.mult,
                          [pk, rk, 'apar'], [xk])
                else:
                    xs, xk = tmp_rot.next()
                    P.act(xs[:, 0:n], pst[:, 0:n], AF.Copy, [pk], [xk])
                p3, p3k = ps_get()
                P.mm(p3[:, 0:n], [(rmat_s[:], xs[:, 0:n])], ['rmat', xk], [p3k])
                b_, bk = tmp_rot.next()
                P.tt('dve', b_[:, 0:n], p3[:, 0:n], csb[:, 512:512 + n], ALU.mult, [p3k, csk], [bk])
                P.tt('pool', xs[:, 0:n], xs[:, 0:n], csb[:, 0:n], ALU.mult, [xk, csk], [xk])
                P.tt('pool', dst[:, t0:t0 + n], xs[:, 0:n], b_[:, 0:n], ALU.add, [xk, bk], [dk])

            import os as _os
            KU = int(_os.environ.get('KU', '8'))
            KST = int(_os.environ.get('KST', '4'))
            for u in range(KU):
                gqa = u < 4
                if gqa:
                    qc, kc, vc, vw, vo = u, 4 + u // 2, 6, 64, (u // 2) * 64
                else:
                    h = u - 4
                    qc, kc, vc, vw, vo = 7 + h, 11 + h, 15 + h, 128, 0
                wq, wqk = wq_rot.next()
                wk, wkk = wk_rot.next()
                wv, wvk = wv_rot.next()
                if KA & 4:
                  P.dma('pool', wq, w_in[ia, qc].rearrange("(p k) f -> p k f", k=8), writes=[wqk])
                if KA & 4:
                  P.dma('pool', wk, w_in[ia, kc].rearrange("(p k) f -> p k f", k=8), writes=[wkk])
                if KA & 4:
                  P.dma('pool', wv, w_in[ia, vc].rearrange("(p k) f -> p k f", k=8), writes=[wvk])
                for (t0, n) in (BLKS if KST >= 1 else []):
                    pst, pk = ps_get()
                    P.mm(pst[:, 0:n], [(wq[:, k, :], xn[:, k, t0:t0 + n]) for k in range(8)], [wqk, ('xn', t0)], [pk])
                    if KST == 1 and int(_os.environ.get('KQ', '1')) == 0:
                        continue
                    qk_post(pst, pk, gqa, 0, qT, ('qT', t0), t0, n)
                    pst, pk = ps_get()
                    P.mm(pst[:, 0:n], [(wk[:, k, :], xn[:, k, t0:t0 + n]) for k in range(8)], [wkk, ('xn', t0)], [pk])
                    qk_post(pst, pk, gqa, 1, kT, ('kT', t0), t0, n)
                if KST < 2:
                    continue
                KV = int(_os.environ.get('KV', '15'))
                if gqa and (KV & 1):
                    P.memset('pool', VA[:, :, 64:128], 0.0, ['VA'])
                    P.memset('pool', VB[:, :, 0:64], 0.0, ['VB'])
                for tt_ in range(18 if (KV & 2) else 0):
                    pst, pk = ps_get()
                    P.mm(pst[:, 0:vw], [(xn[:, k, tt_ * 128:(tt_ + 1) * 128], wv[:, k, vo:vo + vw]) for k in range(8)],
                         [wvk] + xn_keys, [pk])
                    if gqa:
                        if KV & 4:
                            P.act(VA[:, tt_, 0:64], pst[:, 0:64], AF.Copy, [pk], ['VA'])
                        if KV & 8:
                            P.act(VB[:, tt_, 64:128], pst[:, 0:64], AF.Copy, [pk], ['VB'])
                    else:
                        P.act(VA[:, tt_, :], pst[:, 0:128], AF.Copy, [pk], ['VA'])
                kkeys = [('kT', t0) for (t0, n) in BLKS]
                if KST < 3:
                    continue
                for bi, (t0, n) in enumerate(BLKS):
                    nkt = 2 if bi == 0 else 18
                    O1, O1k = ps_tiles[0], ('ps', 0)
                    O2, O2k = ps_tiles[1], ('ps', 1)
                    S1, S1k = ps_tiles[2], ('ps', 2)
                    S2, S2k = ps_tiles[3], ('ps', 3)
                    def emit_s(kt):
                        ia_ = 4 + (2 * kt) % 4
                        sa, sak = ps_tiles[ia_], ('ps', ia_)
                        sbb, sbk = ps_tiles[ia_ + 1], ('ps', ia_ + 1)
                        ks = slice(kt * 128, (kt + 1) * 128)
                        P.mm(sa[:, 0:n], [(kT[0:64, ks], qT[0:64, t0:t0 + n])], kkeys + [('qT', t0)], [sak])
                        P.mm(sbb[:, 0:n], [(kT[64:128, ks], qT[64:128, t0:t0 + n])], kkeys + [('qT', t0)], [sbk])

                    def emit_pv(kt):
                        first, last = kt == 0, kt == nkt - 1
                        ia_ = 4 + (2 * kt) % 4
                        sa, sak = ps_tiles[ia_], ('ps', ia_)
                        sbb, sbk = ps_tiles[ia_ + 1], ('ps', ia_ + 1)
                        pa, pak = tmpb_rot.next()
                        pb, pbk = tmpb_rot.next()
                        P.act(pa[:, 0:n], sa[:, 0:n], AF.Exp, [sak], [pak], scale=0.125)
                        P.act(pb[:, 0:n], sbb[:, 0:n], AF.Exp, [sbk], [pbk], scale=0.125)
                        if gqa:
                            P.mm1(O1[:, 0:n], VA[:, kt, :], pa[:, 0:n], first, False, ['VA', pak], [O1k])
                            P.mm1(O1[:, 0:n], VB[:, kt, :], pb[:, 0:n], False, last, ['VB', pbk], [O1k])
                            P.mm1(S1[:, 0:n], ona_b[:], pa[:, 0:n], first, False, ['ona_b', pak], [S1k])
                            P.mm1(S1[:, 0:n], onb_b[:], pb[:, 0:n], False, last, ['onb_b', pbk], [S1k])
                        else:
                            P.mm1(O1[:, 0:n], VA[:, kt, :], pa[:, 0:n], first, last, ['VA', pak], [O1k])
                            P.mm1(O2[:, 0:n], VA[:, kt, :], pb[:, 0:n], first, last, ['VA', pbk], [O2k])
                            P.mm1(S1[:, 0:n], ones_b[:], pa[:, 0:n], first, last, ['ones_b', pak], [S1k])
                            P.mm1(S2[:, 0:n], ones_b[:], pb[:, 0:n], first, last, ['ones_b', pbk], [S2k])

                    emit_s(0)
                    for kt in range(nkt):
                        if kt + 1 < nkt:
                            emit_s(kt + 1)
                        emit_pv(kt)
                    r1, r1k = tmp_rot.next()
                    P.recip(r1[:, 0:n], S1[:, 0:n], [S1k], [r1k])
                    if gqa:
                        P.tt('dve', merged[:, u, t0:t0 + n], O1[:, 0:n], r1[:, 0:n], ALU.mult, [O1k, r1k], [('mg', t0)])
                    else:
                        r2, r2k = tmp_rot.next()
                        P.recip(r2[:, 0:n], S2[:, 0:n], [S2k], [r2k])
                        P.tt('dve', r1[:, 0:n], O1[:, 0:n], r1[:, 0:n], ALU.mult, [O1k, r1k], [r1k])
                        P.tt('dve', r2[:, 0:n], O2[:, 0:n], r2[:, 0:n], ALU.mult, [O2k, r2k], [r2k])
                        P.stt('dve', r1[:, 0:n], r2[:, 0:n], apar[:, 3:4], r1[:, 0:n], ALU.mult, ALU.add,
                              [r1k, r2k, 'apar'], [r1k])
                        sq, sk = tmpb_rot.next()
                        P.act(sq[:, 0:n], r1[:, 0:n], AF.Square, [r1k], [sk])
                        p2, p2k = ps_tiles[4], ('ps', 4)
                        P.mm(p2[:, 0:n], [(ones_b[:], sq[:, 0:n])], ['ones_b', sk], [p2k])
                        P.act(r2[:, 0:n], p2[:, 0:n], AF.Sqrt, [p2k], [r2k], bias=EPS, scale=1.0 / 128)
                        P.recip(r2[:, 0:n], r2[:, 0:n], [r2k], [r2k])
                        P.stt('dve', merged[:, u, t0:t0 + n], r1[:, 0:n], apar[:, 2:3], r2[:, 0:n], ALU.mult, ALU.mult,
                              [r1k, r2k, 'apar'], [('mg', t0)])
            for bi, (t0, n) in enumerate(BLKS if KST >= 4 else []):
                j = 1 if bi == 0 else 0

                def get_ps(oc, t0=t0, n=n):
                    po, pok = ps_get()
                    P.mm(po[:, 0:n], [(wo[:, k, oc * 128:(oc + 1) * 128], merged[:, k, t0:t0 + n]) for k in range(8)],
                         ['wo', ('mg', t0)], [pok])
                    return po[:, 0:n], [pok]
                add_residual(hb_rot, t0, n, j, 16, get_ps)
            P.barrier()

        def phase_ssm(l):
            i_s = l // 2
            C = Carver(arena)
            TT = 256
            prm = C.f(32 * 20).rearrange("p (a b) -> p a b", b=32)
            c16 = lambda t: t.rearrange("p (a c) -> p a c", c=16)
            st4 = lambda t: t.rearrange("p (s t) -> p s t", t=TT)
            Wp = [C.f(64).rearrange("p (a b) -> p a b", b=32) for _ in range(2)]
            RRp = [C.f(32) for _ in range(2)]
            Bpad = [C.b(64 * 128).rearrange("p (s c) -> p s c", c=128) for _ in range(2)]
            Cpad = [C.b(64 * 128).rearrange("p (s c) -> p s c", c=128) for _ in range(2)]
            o_work = C.o
            bre, bim, cre, cim, bbr, bbi = [c16(C.f(512)) for _ in range(6)]
            ych = C.f(128)
            K = ['ssmp']
            P.dma('sp', dsk[:], s_d[i_s], writes=['dsk'])

            for d in range(2):
                are, aim, ldt = prm[:, 0, :], prm[:, 1, :], prm[:, 2, :]
                P.dma('sp', are, s_are[i_s, d], writes=K)
                P.dma('sp', aim, s_aim[i_s, d], writes=K)
                P.dma('sp', ldt, s_ldt[i_s, d], writes=K)
                P.dma('sp', bre, s_bre[i_s, d], writes=K)
                P.dma('sp', bim, s_bim[i_s, d], writes=K)
                P.dma('sp', cre, s_cre[i_s, d], writes=K)
                P.dma('sp', cim, s_cim[i_s, d], writes=K)
                lre, dt, zr, zi, t1, t2, nr, den, cr_, ci_, rr, wr, wi, u1, u2 = [prm[:, 3 + q, :] for q in range(15)]
                P.ts1('dve', lre, are, -1e-4, ALU.min, K, K)
                P.act(dt, ldt, AF.Exp, K, K)
                P.tt('dve', t1, lre, dt, ALU.mult, K, K)
                P.tt('dve', t2, aim, dt, ALU.mult, K, K)
                P.act(rr, t1, AF.Exp, K, K)
                P.act(zr, t1, AF.Exp, K, K, scale=1.0 / 16)
                P.act(zi, t2, AF.Sin, K, K, scale=1.0 / 16)
                P.ts('dve', t2, t2, 1.0 / 16, math.pi / 2, ALU.mult, ALU.add, K, K)
                P.act(t2, t2, AF.Sin, K, K)
                P.cp('dve', wi, zi, K, K)
                P.cp('dve', wr, t2, K, K)
                P.tt('dve', zi, zi, zr, ALU.mult, K, K)
                P.tt('dve', zr, t2, zr, ALU.mult, K, K)
                for _ in range(4):
                    P.tt('dve', t1, zr, zr, ALU.mult, K, K)
                    P.tt('dve', t2, zi, zi, ALU.mult, K, K)
                    P.tt('dve', zi, zr, zi, ALU.mult, K, K)
                    P.ts1('dve', zi, zi, 2.0, ALU.mult, K, K)
                    P.tt('dve', zr, t1, t2, ALU.subtract, K, K)
                def wsq():
                    P.tt('dve', u1, wr, wr, ALU.mult, K, K)
                    P.tt('dve', u2, wi, wi, ALU.mult, K, K)
                    P.tt('dve', wi, wr, wi, ALU.mult, K, K)
                    P.ts1('dve', wi, wi, 2.0, ALU.mult, K, K)
                    P.tt('dve', wr, u1, u2, ALU.subtract, K, K)
                for _ in range(4):
                    wsq()
                P.cp('dve', Wp[d][:, 0, :], wr, K, K)
                P.cp('dve', Wp[d][:, 1, :], wi, K, K)
                P.cp('dve', RRp[d], rr, K, K)
                P.ts1('dve', nr, zr, -1.0, ALU.add, K, K)
                P.tt('dve', den, lre, lre, ALU.mult, K, K)
                P.tt('dve', t1, aim, aim, ALU.mult, K, K)
                P.tt('dve', den, den, t1, ALU.add, K, K)
                P.recip(den, den, K, K)
                P.tt('dve', t1, nr, lre, ALU.mult, K, K)
                P.tt('dve', t2, zi, aim, ALU.mult, K, K)
                P.tt('dve', cr_, t1, t2, ALU.add, K, K)
                P.tt('dve', cr_, cr_, den, ALU.mult, K, K)
                P.tt('dve', t1, zi, lre, ALU.mult, K, K)
                P.tt('dve', t2, nr, aim, ALU.mult, K, K)
                P.tt('dve', ci_, t1, t2, ALU.subtract, K, K)
                P.tt('dve', ci_, ci_, den, ALU.mult, K, K)
                crb = cr_.unsqueeze(2).to_broadcast([128, 32, 16])
                cib = ci_.unsqueeze(2).to_broadcast([128, 32, 16])
                P.tt('dve', bbr, bre, crb, ALU.mult, K, K)
                P.tt('dve', bbi, bim, cib, ALU.mult, K, K)
                P.tt('dve', bbr, bbr, bbi, ALU.subtract, K, K)
                P.tt('dve', bbi, bre, cib, ALU.mult, K, K)
                P.tt('dve', bre, bim, crb, ALU.mult, K, K)
                P.tt('dve', bbi, bbi, bre, ALU.add, K, K)
                for dc in range(8):
                    for ri, src in ((0, bbr), (1, bbi)):
                        yv = ych.rearrange("p (q g c) -> p q g c", q=4, g=2)
                        for g2 in range(2):
                            P.ts1('dve', yv[:, :, g2, :], src[:, dc * 4:(dc + 1) * 4, :], pm[:, g2:g2 + 1],
                                  ALU.mult, K + ['pm', 'ych'], ['ych'])
                        pt, ptk = ps_get()
                        P.tr(pt[:, 0:128], ych, ident_s[:], ['ych', 'ident'], [ptk])
                        for q in range(4):
                            P.ts1('dve', Bpad[d][:, dc * 8 + q * 2 + ri, :], pt[:, 0:128], pm[:, 4 + q:5 + q],
                                  ALU.mult, [ptk, 'pm'], [('Bpad', d)])
                P.memset('pool', Cpad[d], 0.0, [('Cpad', d)])
                for pr in range(32):
                    dc, q = pr // 4, pr % 4
                    for ri, src, mo in ((0, cre, 0), (1, cim, 2)):
                        cv = Cpad[d][:, dc * 8 + q * 2 + ri, :].rearrange("p (q g c) -> p q g c", q=4, g=2)
                        for g2 in range(2):
                            P.ts1('pool', cv[:, q, g2, :], src[:, pr, :], pm[:, mo + g2:mo + g2 + 1],
                                  ALU.mult, K + ['pm'], [('Cpad', d)])

            def blocks_for(d):
                if d == 0:
                    return [(b * TT, False) for b in range(NT // TT)]
                return [(0, True)] + [(b * TT, True) for b in range(NT // TT - 1, 0, -1)]
            seqs = [blocks_for(0), blocks_for(1)]
            P.barrier()
            C.o = o_work
            COSa = st4(C.f(8 * TT))
            SINa = st4(C.f(8 * TT))
            RTa = st4(C.f(8 * TT))
            COSb = [COSa[:, d_ * 4:(d_ + 1) * 4, :] for d_ in range(2)]
            SINb = [SINa[:, d_ * 4:(d_ + 1) * 4, :] for d_ in range(2)]
            RTb = [RTa[:, d_ * 4:(d_ + 1) * 4, :] for d_ in range(2)]
            R2b = [C.f(8) for _ in range(2)]
            pw = C.f(32).rearrange("p (a b) -> p a b", b=8)
            BUs = [st4(C.f(8 * TT)) for _ in range(2)]
            Qin = [st4(C.f(8 * TT)) for _ in range(2)]
            tm = [st4(C.f(4 * TT)) for _ in range(4)]
            tmk = [('stm', i) for i in range(4)]
            Sb = [st4(C.b(8 * TT)) for _ in range(2)]
            spv = [C.f(8) for _ in range(2)]
            fx = [C.f(8) for _ in range(2)]
            yd = [C.f(TT) for _ in range(2)]

            def build_tab(dc):
                K2 = [('tab', 0), ('tab', 1)]
                p4 = slice(dc * 4, dc * 4 + 4)
                wr_, wi_, u1_, u2_ = [pw[:, i, :] for i in range(4)]
                for d_ in range(2):
                    P.cp('dve', wr_[:, d_ * 4:(d_ + 1) * 4], Wp[d_][:, 0, p4], [], K2)
                    P.cp('dve', wi_[:, d_ * 4:(d_ + 1) * 4], Wp[d_][:, 1, p4], [], K2)
                P.cp('dve', COSa[:, :, 0], wr_, K2, K2)
                P.cp('dve', SINa[:, :, 0], wi_, K2, K2)
                ta = tm[0].rearrange("p s t -> p (s t)")[:, 0:8 * (TT // 2)].rearrange("p (s t) -> p s t", t=TT // 2)
                tb = tm[1].rearrange("p s t -> p (s t)")[:, 0:8 * (TT // 2)].rearrange("p (s t) -> p s t", t=TT // 2)
                kk = 1
                while kk < TT:
                    cb = wr_.unsqueeze(2).to_broadcast([128, 8, kk])
                    sb_ = wi_.unsqueeze(2).to_broadcast([128, 8, kk])
                    c0 = COSa[:, :, 0:kk]
                    s0 = SINa[:, :, 0:kk]
                    a_ = ta[:, :, 0:kk]
                    b_ = tb[:, :, 0:kk]
                    P.tt('dve', a_, c0, cb, ALU.mult, K2, [tmk[0]])
                    P.tt('dve', b_, s0, sb_, ALU.mult, K2, [tmk[1]])
                    P.tt('dve', COSa[:, :, kk:2 * kk], a_, b_, ALU.subtract, [tmk[0], tmk[1]], K2)
                    P.tt('dve', a_, s0, cb, ALU.mult, K2, [tmk[0]])
                    P.tt('dve', b_, c0, sb_, ALU.mult, K2, [tmk[1]])
                    P.tt('dve', SINa[:, :, kk:2 * kk], a_, b_, ALU.add, [tmk[0], tmk[1]], K2)
                    kk *= 2
                    if kk < TT:
                        P.tt('dve', u1_, wr_, wr_, ALU.mult, K2, K2)
                        P.tt('dve', u2_, wi_, wi_, ALU.mult, K2, K2)
                        P.tt('dve', wi_, wr_, wi_, ALU.mult, K2, K2)
                        P.ts1('dve', wi_, wi_, 2.0, ALU.mult, K2, K2)
                        P.tt('dve', wr_, u1_, u2_, ALU.subtract, K2, K2)
                for d_ in range(2):
                    P.cp('dve', RTb[d_], RRp[d_][:, p4].unsqueeze(2).to_broadcast([128, 4, TT]), [], K2)
                    P.cp('dve', R2b[d_][:, 0:4], RRp[d_][:, p4], [], K2)
                    P.cp('dve', R2b[d_][:, 4:8], RRp[d_][:, p4], [], K2)
                P.memset('dve', RTa[:, :, 0:1], 0.0, K2)

            for dc in range(8):
                build_tab(dc)
                for bidx in range(NT // TT):
                    def stage_ab(d):
                        t0, rev = seqs[d][bidx]
                        kT_ = ('tab', d)
                        kBr, kBi, kQr, kQi = ('BUr', d), ('BUi', d), ('Qr', d), ('Qi', d)
                        for ri in range(2):
                            for qh in range(2):
                                pst, pk = ps_get()
                                for q2 in range(2):
                                    q = qh * 2 + q2
                                    P.mm1(pst[:, q2 * TT:(q2 + 1) * TT], Bpad[d][:, dc * 8 + q * 2 + ri, :],
                                          xn[:, dc, t0:t0 + TT], True, True, xn_keys, [pk])
                                src = pst[:, 0:512].rearrange("p (s t) -> p s t", t=TT)
                                if rev:
                                    src = src[:, :, ::-1]
                                ls = ri * 4 + qh * 2
                                P.act(BUs[d][:, ls:ls + 2, :], src, AF.Copy, [pk], [kBr if ri == 0 else kBi])
                        Bre, Bim = BUs[d][:, 0:4, :], BUs[d][:, 4:8, :]
                        Qr, Qi = Qin[d][:, 0:4, :], Qin[d][:, 4:8, :]
                        P.tt('pool', tm[2 + d], Bre, SINb[d], ALU.mult, [kBr, kT_], [tmk[2 + d]])
                        P.tt('dve', Qi, Bim, COSb[d], ALU.mult, [kBi, kT_], [kQi])
                        P.tt('pool', Qi, Qi, tm[2 + d], ALU.subtract, [kQi, tmk[2 + d]], [kQi])
                        P.tt('dve', tm[0], Bre, COSb[d], ALU.mult, [kBr, kT_], [tmk[0]])
                        P.tt('dve', tm[1], Bim, SINb[d], ALU.mult, [kBi, kT_], [tmk[1]])
                        P.tt('dve', Qr, tm[0], tm[1], ALU.add, [tmk[0], tmk[1]], [kQr])

                    def stage_c(d):
                        kT_ = ('tab', d)
                        kQr, kQi = ('Qr', d), ('Qi', d)
                        if bidx > 0:
                            P.tt('dve', fx[d], spv[d], R2b[d], ALU.mult, [('sp', d), kT_], [('fx', d)])
                            q0 = Qin[d][:, :, 0]
                            P.tt('dve', q0, q0, fx[d], ALU.add, [kQr, kQi, ('fx', d)], [kQr, kQi])
                        rt2 = RTb[d].rearrange("p s t -> p (s t)")
                        for h, kq in ((0, kQr), (1, kQi)):
                            v = Qin[d][:, h * 4:(h + 1) * 4, :].rearrange("p s t -> p (s t)")
                            P.op('dve', lambda e, v=v, rt2=rt2: e.tensor_tensor_scan(v, rt2, v, 0.0, ALU.mult, ALU.add),
                                 [kq, kT_], [kq])

                    def stage_d(d):
                        kT_ = ('tab', d)
                        kBr, kBi, kQr, kQi = ('BUr', d), ('BUi', d), ('Qr', d), ('Qi', d)
                        Bre, Bim = BUs[d][:, 0:4, :], BUs[d][:, 4:8, :]
                        Qr, Qi = Qin[d][:, 0:4, :], Qin[d][:, 4:8, :]
                        P.tt('pool', tm[2 + d], Qr, SINb[d], ALU.mult, [kQr, kT_], [tmk[2 + d]])
                        P.tt('dve', Bim, Qi, COSb[d], ALU.mult, [kQi, kT_], [kBi])
                        P.tt('pool', Bim, Bim, tm[2 + d], ALU.add, [kBi, tmk[2 + d]], [kBi])
                        P.tt('dve', tm[0], Qr, COSb[d], ALU.mult, [kQr, kT_], [tmk[0]])
                        P.tt('dve', tm[1], Qi, SINb[d], ALU.mult, [kQi, kT_], [tmk[1]])
                        P.tt('dve', Bre, tm[0], tm[1], ALU.subtract, [tmk[0], tmk[1]], [kBr])
                        P.cp('pool', spv[d], BUs[d][:, :, TT - 1], [kBr, kBi], [('sp', d)])

                    def stage_ef(d):
                        t0, rev = seqs[d][bidx]
                        kBr, kBi = ('BUr', d), ('BUi', d)
                        src = BUs[d][:, :, ::-1] if rev else BUs[d]
                        P.act(Sb[d], src, AF.Copy, [kBr, kBi], [('Sb', d)])
                        pst, pk = ps_get()
                        prs = []
                        for q in range(4):
                            for ri in range(2):
                                prs.append((Cpad[d][:, dc * 8 + q * 2 + ri, :], Sb[d][:, ri * 4 + q, :]))
                        P.mm(pst[:, 0:TT], prs, [('Sb', d)], [pk])
                        P.act(yd[d], pst[:, 0:TT], AF.Copy, [pk], [('yd', d)])
                        P.dma('sp', y3[d][:, dc, t0:t0 + TT], yd[d], reads=[('yd', d)], writes=[('y', d)])

                    stage_ab(0)
                    stage_ab(1)
                    stage_c(0)
                    stage_c(1)
                    stage_d(0)
                    stage_d(1)
                    stage_ef(0)
                    stage_ef(1)
            P.barrier()

            C = Carver(arena)
            wa = k8(C.b(8192))
            wb = k8(C.b(8192))
            yf_rot = mkrot(C, "yfb", 1, 4096, k8)
            yb_rot = mkrot(C, "ybb", 1, 4096, k8)
            vb_rot = mkrot(C, "vbf", 1, 4096, k8, bf=True)
            hb_rot = mkrot(C, "shb", 2, 4096, k8)
            tmp_rot = mkrot(C, "stmpg", 3, 512)
            P.dma('pool', wa, glu_a[i_s].rearrange("(k p) f -> p k f", p=128), writes=['wa'])
            P.dma('pool', wb, glu_b[i_s].rearrange("(k p) f -> p k f", p=128), writes=['wb'])
            for bi, (t0, n) in enumerate(BLKS):
                j = 1 if bi == 0 else 0
                yfb, yfk = yf_rot.next()
                ybb, ybk = yb_rot.next()
                vbf, vbk = vb_rot.next()
                P.dma('sp', yfb[:, :, 0:n], y3[0][:, :, t0:t0 + n], writes=[yfk])
                P.dma('sp', ybb[:, :, 0:n], y3[1][:, :, t0:t0 + n], writes=[ybk])
                P.tt('dve', yfb[:, :, 0:n], yfb[:, :, 0:n], ybb[:, :, 0:n], ALU.add, [yfk, ybk], [yfk])
                for k in range(8):
                    P.stt('dve', yfb[:, k, 0:n], xn[:, k, t0:t0 + n], dsk[:, k:k + 1], yfb[:, k, 0:n], ALU.mult, ALU.add,
                          [yfk, 'dsk', ('xn', t0)], [yfk])
                P.act(vbf[:, :, 0:n], yfb[:, :, 0:n], AF.Gelu, [yfk], [vbk])

                def get_ps(oc, t0=t0, n=n, vbf=vbf, vbk=vbk):
                    pa_, pak_ = ps_get()
                    pb_, pbk_ = ps_get()
                    P.mm(pa_[:, 0:n], [(wa[:, k, oc * 128:(oc + 1) * 128], vbf[:, k, 0:n]) for k in range(8)],
                         ['wa', vbk], [pak_])
                    P.mm(pb_[:, 0:n], [(wb[:, k, oc * 128:(oc + 1) * 128], vbf[:, k, 0:n]) for k in range(8)],
                         ['wb', vbk], [pbk_])
                    sg, sgk = tmp_rot.next()
                    P.act(sg[:, 0:n], pb_[:, 0:n], AF.Sigmoid, [pbk_], [sgk])
                    P.tt('dve', sg[:, 0:n], pa_[:, 0:n], sg[:, 0:n], ALU.mult, [pak_, sgk], [sgk])
                    return sg[:, 0:n], [sgk]
                add_residual(hb_rot, t0, n, j, 16, get_ps)
            P.barrier()

        import os as _os
        kph = int(_os.environ.get('KPH', '4'))
        for l in range(NLAYERS):
            phase_mod(l)
            if kph >= 2:
                phase_norm1()
            if kph >= 3:
                if l % 2 == 0:
                    phase_attn(l)
                else:
                    phase_ssm(l)
            if kph >= 4:
                phase_moe(l)

        C = Carver(arena)
        hb_rot = mkrot(C, "fhb", 2, 4096, k8)
        sq_rot = mkrot(C, "fsq", 2, 4096, k8, bf=True)
        rs_rot = mkrot(C, "frs", 2, 512)
        for bi, (t0, n) in enumerate(BLKS):
            if bi == 0:
                continue
            hb, hk = hb_rot.next()
            P.dma('sp', hb[:, :, 0:n], hT3[:, :, t0:t0 + n], reads=[('h', t0)], writes=[hk])
            sq, sk = sq_rot.next()
            P.act(sq[:, :, 0:n], hb[:, :, 0:n], AF.Square, [hk], [sk])
            pst, pk = ps_get()
            P.mm(pst[:, 0:n], [(ones_b[:], sq[:, k, 0:n]) for k in range(8)], ['ones_b', sk], [pk])
            rs, rk = rs_rot.next()
            P.act(rs[:, 0:n], pst[:, 0:n], AF.Sqrt, [pk], [rk], bias=EPS, scale=1.0 / 1024)
            P.recip(rs[:, 0:n], rs[:, 0:n], [rk], [rk])
            for k in range(8):
                P.stt('dve', hb[:, k, 0:n], hb[:, k, 0:n], fg_s[:, k:k + 1], rs[:, 0:n], ALU.mult, ALU.mult,
                      [hk, 'fg', rk], [hk])
            P.dma('sp', outT3[:, :, t0 - NCX:t0 - NCX + n], hb[:, :, 0:n], reads=[hk], writes=[('out', t0)])
        P.finish('sp')
        P.emit()
    return nc


def _pk(v):
    return np.ascontiguousarray(v.reshape(8, 128).T)


def _host_consts():
    f32 = np.float32
    half = 32
    inv = (1.0 / (10000.0 ** (np.arange(0, half, 2, dtype=f32) / half))).astype(f32)
    t = np.arange(2048)
    rows = (t // 64).astype(f32)
    cols = (t % 64).astype(f32)
    ang_r = rows[:, None] * inv
    ang_c = cols[:, None] * inv
    ang = np.concatenate([ang_r, ang_r, ang_c, ang_c], axis=-1).astype(f32)
    cos = np.cos(ang).astype(f32)
    sin = np.sin(ang).astype(f32)
    cosT = np.ones((128, NT), f32)
    sinT = np.zeros((128, NT), f32)
    cosT[:, NCX:] = np.tile(cos.T, (2, 1))
    sinT[:, NCX:] = np.tile(sin.T, (2, 1))
    R = np.zeros((64, 64), f32)
    for s in (0, 32):
        for i in range(16):
            R[s + i, s + 16 + i] = -1.0
            R[s + 16 + i, s + i] = 1.0
    Rb = np.zeros((128, 128), f32)
    Rb[0:64, 0:64] = R
    Rb[64:128, 64:128] = R
    rmat = np.ascontiguousarray(Rb.T)
    ident = np.eye(128, dtype=f32)
    bones = np.zeros((128, 128), f32)
    bones[0:64, 0:64] = 1.0
    bones[64:128, 64:128] = 1.0
    pmask = np.zeros((128, 8), f32)
    pmask[0:64, 0] = 1.0
    pmask[64:128, 1] = 1.0
    pmask[0:64, 2] = -1.0
    pmask[64:128, 3] = -1.0
    for q in range(4):
        pmask[q * 32:(q + 1) * 32, 4 + q] = 1.0
    return dict(cosT=cosT, sinT=sinT, rmat=rmat, ident=ident, bones=bones, pmask=pmask)


_NC_CACHE = {}


def kernel(x, c, ctx, c_ctx, mod_w, mod_b, norm1_g, norm2_g, final_g,
           attn_w_in, attn_w_out, attn_q_norm_g, attn_k_norm_g,
           diff_lambda_q1, diff_lambda_k1, diff_lambda_q2, diff_lambda_k2, diff_subln_g,
           ssm_a_re, ssm_a_im, ssm_log_dt, ssm_b_re, ssm_b_im, ssm_c_re, ssm_c_im, ssm_d,
           ssm_glu_w_a, ssm_glu_w_b,
           moe_group_w, moe_group_b, moe_router_w, moe_router_b, moe_w_gate, moe_w_up, moe_w_down):
    f32 = np.float32
    A = lambda a: np.ascontiguousarray(np.asarray(a, dtype=f32))
    x, c, ctx, c_ctx = A(x), A(c), A(ctx), A(c_ctx)
    shared = _host_consts()
    shared["mod_w"] = A(mod_w)
    shared["mod_bT"] = A(np.asarray(mod_b).reshape(4, 48, 128).transpose(0, 2, 1))
    shared["n1g"] = A(np.asarray(norm1_g).reshape(4, 8, 128).transpose(0, 2, 1))
    shared["n2g"] = A(np.asarray(norm2_g).reshape(4, 8, 128).transpose(0, 2, 1))
    shared["fgT"] = _pk(np.asarray(final_g, dtype=f32))
    wi = np.asarray(attn_w_in, dtype=f32)
    qa, ka, va, qb, kb, vb = (wi[:, :, 0:512], wi[:, :, 512:640], wi[:, :, 640:768], wi[:, :, 768:1280],
                              wi[:, :, 1280:1792], wi[:, :, 1792:2304])
    wcat = np.concatenate([qa, ka[:, :, 0:64], ka[:, :, 0:64], ka[:, :, 64:128], ka[:, :, 64:128],
                           va, qb, kb, vb], axis=2)
    shared["w_in"] = A(wcat.reshape(2, 8, 128, 19, 128).transpose(0, 3, 2, 1, 4).reshape(2, 19, 1024, 128))
    shared["w_out"] = A(attn_w_out)
    shared["qg"] = A(np.tile(np.asarray(attn_q_norm_g, dtype=f32), (1, 2))[:, :, None])
    shared["kg"] = A(np.tile(np.asarray(attn_k_norm_g, dtype=f32), (1, 2))[:, :, None])
    shared["slg"] = A(np.asarray(diff_subln_g, dtype=f32)[:, :, None])
    lv = np.stack([np.asarray(a, dtype=f32) for a in (diff_lambda_q1, diff_lambda_k1, diff_lambda_q2, diff_lambda_k2)], axis=1)
    shared["lamv"] = A(np.broadcast_to(lv[:, None, :, :], (2, 128, 4, 64)))

    def LL(a):
        a = np.asarray(a, dtype=f32).reshape(2, 2, 32, 2, 64)
        return A(a.transpose(0, 1, 3, 4, 2).reshape(2, 2, 128, 32))
    shared["s_are"] = LL(ssm_a_re)
    shared["s_aim"] = LL(ssm_a_im)
    shared["s_ldt"] = LL(np.broadcast_to(np.asarray(ssm_log_dt, dtype=f32)[:, :, :, None], (2, 2, 64, 64)))

    def LB(a):
        a = np.asarray(a, dtype=f32).reshape(2, 2, 32, 2, 64, 16)
        return A(a.transpose(0, 1, 3, 4, 2, 5).reshape(2, 2, 128, 32, 16))

    def LC(a):
        a = np.asarray(a, dtype=f32).reshape(2, 2, 32, 2, 16, 64)
        return A(a.transpose(0, 1, 3, 5, 2, 4).reshape(2, 2, 128, 32, 16))
    shared["s_bre"] = LB(ssm_b_re)
    shared["s_bim"] = LB(ssm_b_im)
    shared["s_cre"] = LC(ssm_c_re)
    shared["s_cim"] = LC(ssm_c_im)
    shared["s_d"] = A(np.asarray(ssm_d, dtype=f32).reshape(2, 8, 128).transpose(0, 2, 1))
    shared["glu_a"] = A(ssm_glu_w_a)
    shared["glu_b"] = A(ssm_glu_w_b)
    gw = np.asarray(moe_group_w, dtype=f32)
    rw = np.asarray(moe_router_w, dtype=f32)
    shared["wr"] = A(np.concatenate([gw, rw.transpose(0, 2, 1, 3).reshape(4, 1024, 32)], axis=2))
    rb = np.concatenate([np.asarray(moe_group_b, dtype=f32), np.asarray(moe_router_b, dtype=f32).reshape(4, 32)], axis=1)
    shared["wrb"] = A(np.broadcast_to(rb[:, None, :], (4, 128, 36)))
    pk8 = lambda w: A(np.asarray(w, dtype=f32).reshape(4, 32, 8, 128, 256).transpose(0, 1, 3, 2, 4).reshape(4, 32, 1024, 256))
    shared["w_gate"] = pk8(moe_w_gate)
    shared["w_up"] = pk8(moe_w_up)
    shared["w_down"] = A(moe_w_down)

    if "nc" not in _NC_CACHE:
        _NC_CACHE["nc"] = build()
    nc = _NC_CACHE["nc"]
    in_maps = []
    import os as _os
    ncores = int(_os.environ.get('KCORES', '8'))
    for b in range(ncores):
        m = dict(shared)
        m["xT"] = A(np.concatenate([ctx[b], x[b]], axis=0).T)
        cc = np.stack([c[b], c_ctx], axis=1)
        m["cT"] = A(cc.reshape(8, 128, 2).transpose(1, 0, 2))
        in_maps.append(m)
    res = run_bass_kernel_spmd(nc, in_maps, core_ids=list(range(ncores)))
    out = np.stack([np.ascontiguousarray(r["outT"].T) for r in res.results], axis=0)
    return out.astype(np.float32)
```

```python
import contextlib
import math
import numpy as np
import concourse.bass as bass
import concourse.mybir as mybir
from concourse.bass_utils import run_bass_kernel_spmd

F32 = mybir.dt.float32
BF16 = mybir.dt.bfloat16
ALU = mybir.AluOpType
AF = mybir.ActivationFunctionType
AX = mybir.AxisListType

EPOCH = 30000
NDMASEM = 24
DEPTH = 4
NT = 2304
NCX = 256
EPS = 1e-6
BLKS = [(0, 256)] + [(256 + 512 * i, 512) for i in range(4)]
TB = 32
NLAYERS = 4


class Prog:
    def __init__(self, nc, stack):
        self.nc = nc
        self.stack = stack
        self.engs = {'pe': nc.tensor, 'act': nc.scalar, 'dve': nc.vector,
                     'pool': nc.gpsimd, 'sp': nc.sync}
        self.streams = {e: [] for e in self.engs}
        self.cnt = {e: 0 for e in self.engs}
        self.esems = {e: [] for e in self.engs}
        self.known = {e: {} for e in self.engs}
        self.last_w = {}
        self.readers = {}
        self.dsems = []
        self.dcnt = []
        self.dnext = [0, 0]
        self.nsem = 0
        self.pesem = set()
        self.psi = 0

    def _newsem(self, name):
        self.nsem += 1
        return self.stack.enter_context(self.nc.semaphore(name))

    def _esem(self, e):
        k = self.cnt[e] // EPOCH
        while len(self.esems[e]) <= k:
            s = self._newsem(f"s_{e}_{len(self.esems[e])}")
            self.esems[e].append(s)
            if e == 'pe':
                self.pesem.add(id(s))
        return self.esems[e][k], self.cnt[e] % EPOCH + 1

    def _deps(self, e, reads, writes):
        deps = {}

        def add(t):
            if t is None:
                return
            s, v = t
            if e == 'pe' and id(s) in self.pesem:
                return
            if deps.get(id(s), (s, 0))[1] < v:
                deps[id(s)] = (s, v)
        for k in reads:
            add(self.last_w.get(k))
        for k in writes:
            add(self.last_w.get(k))
            for t in self.readers.get(k, ()):
                add(t)
        out = []
        kn = self.known[e]
        for sid, (s, v) in deps.items():
            if kn.get(sid, 0) < v:
                kn[sid] = v
                out.append((s, v))
        return out

    def _mark(self, reads, writes, tok):
        for k in reads:
            self.readers.setdefault(k, []).append(tok)
        for k in writes:
            self.last_w[k] = tok
            self.readers[k] = []

    def op(self, e, fn, reads=(), writes=()):
        waits = self._deps(e, reads, writes)
        sem, val = self._esem(e)
        self.cnt[e] += 1

        def run(eng, waits=waits, fn=fn, sem=sem):
            for s, v in waits:
                eng.wait_ge(s, v)
            fn(eng).then_inc(sem, 1)
        self.streams[e].append(run)
        self._mark(reads, writes, (sem, val))

    def dma(self, e, out, in_, reads=(), writes=()):
        lo, n = (0, NDMASEM - 8) if e != 'pool' else (NDMASEM - 8, 8)
        while len(self.dsems) < NDMASEM:
            self.dsems.append(self._newsem(f"s_dma_{len(self.dsems)}"))
            self.dcnt.append(0)
        i = lo + self.dnext[e != 'pool']
        self.dnext[e != 'pool'] = (self.dnext[e != 'pool'] + 1) % n
        if self.dcnt[i] * 16 >= 30000:
            self.dsems[i] = self._newsem(f"s_dma_{i}_x{self.nsem}")
            self.dcnt[i] = 0
        sem = self.dsems[i]
        waits = self._deps(e, reads, writes)
        prev = self.dcnt[i] * 16
        kn = self.known[e]
        if prev and kn.get(id(sem), 0) < prev:
            kn[id(sem)] = prev
            waits.append((sem, prev))
        self.dcnt[i] += 1
        val = self.dcnt[i] * 16

        def run(eng, waits=waits, sem=sem):
            for s, v in waits:
                eng.wait_ge(s, v)
            eng.dma_start(out=out, in_=in_).then_inc(sem, 16)
        self.streams[e].append(run)
        self._mark(reads, writes, (sem, val))

    def barrier(self):
        allw = []
        for i, sm in enumerate(self.dsems):
            if self.dcnt[i]:
                allw.append((sm, self.dcnt[i] * 16))
        for en in self.engs:
            if self.cnt[en]:
                k = (self.cnt[en] - 1) // EPOCH
                allw.append((self.esems[en][k], (self.cnt[en] - 1) % EPOCH + 1))
        for e in self.engs:
            kn = self.known[e]
            waits = []
            for sm, v in allw:
                if kn.get(id(sm), 0) < v:
                    kn[id(sm)] = v
                    waits.append((sm, v))

            def run(eng, waits=waits):
                for sm, v in waits:
                    eng.wait_ge(sm, v)
            self.streams[e].append(run)
        self.last_w.clear()
        self.readers.clear()

    def finish(self, e='sp'):
        waits = []
        for i, s in enumerate(self.dsems):
            if self.dcnt[i]:
                waits.append((s, self.dcnt[i] * 16))
        for en in self.engs:
            if self.cnt[en] and en != e:
                k = (self.cnt[en] - 1) // EPOCH
                waits.append((self.esems[en][k], (self.cnt[en] - 1) % EPOCH + 1))

        def run(eng):
            for s, v in waits:
                eng.wait_ge(s, v)
        self.streams[e].append(run)

    def emit(self):
        with self.nc.Block() as block:
            @block.sync
            def _(eng):
                for c in self.streams['sp']:
                    c(eng)

            @block.tensor
            def _(eng):
                for c in self.streams['pe']:
                    c(eng)

            @block.scalar
            def _(eng):
                for c in self.streams['act']:
                    c(eng)

            @block.vector
            def _(eng):
                for c in self.streams['dve']:
                    c(eng)

            @block.gpsimd
            def _(eng):
                for c in self.streams['pool']:
                    c(eng)

    def mm(self, out, pairs, reads, writes):
        def fn(e, out=out, pairs=pairs):
            n = len(pairs)
            ins = None
            for i, (l, r) in enumerate(pairs):
                ins = e.matmul(out, l, r, start=(i == 0), stop=(i == n - 1))
            return ins
        self.op('pe', fn, reads, writes)

    def mm1(self, out, l, r, start, stop, reads, writes):
        self.op('pe', lambda e: e.matmul(out, l, r, start=start, stop=stop), reads, writes)

    def tr(self, out, in_, ident, reads, writes):
        self.op('pe', lambda e: e.transpose(out, in_, ident), reads, writes)

    def act(self, out, in_, func, reads, writes, **kw):
        self.op('act', lambda e: e.activation(out, in_, func, **kw), reads, writes)

    def tt(self, eng, out, a, b, op, reads, writes):
        self.op(eng, lambda e: e.tensor_tensor(out, a, b, op), reads, writes)

    def ts(self, eng, out, a, s1, s2, op0, op1, reads, writes):
        self.op(eng, lambda e: e.tensor_scalar(out, a, s1, s2, op0, op1), reads, writes)

    def ts1(self, eng, out, a, s, op, reads, writes):
        self.op(eng, lambda e: e.tensor_single_scalar(out, a, s, op), reads, writes)

    def stt(self, eng, out, a, s, b, op0, op1, reads, writes):
        self.op(eng, lambda e: e.scalar_tensor_tensor(out, a, s, b, op0, op1), reads, writes)

    def cp(self, eng, out, a, reads, writes):
        self.op(eng, lambda e: e.tensor_copy(out, a), reads, writes)

    def memset(self, eng, ap, v, writes):
        self.op(eng, lambda e: e.memset(ap, v), (), writes)

    def recip(self, out, a, reads, writes):
        self.op('dve', lambda e: e.reciprocal(out, a), reads, writes)


class Rot:
    def __init__(self, tiles, name):
        self.tiles = tiles
        self.name = name
        self.i = 0

    def next(self):
        i = self.i
        self.i = (i + 1) % len(self.tiles)
        return self.tiles[i], (self.name, i)


ARENA = 39000


class Carver:
    def __init__(self, arena):
        self.arena = arena
        self.o = 0
        self.bf = arena[:].bitcast(BF16)

    def f(self, n):
        a = self.o
        self.o += n
        assert self.o <= ARENA, self.o
        return self.arena[:, a:a + n]

    def b(self, n):
        n2 = (n + 1) // 2
        a = self.o
        self.o += n2
        assert self.o <= ARENA, self.o
        return self.bf[:, 2 * a:2 * a + n]


class Rot:
    def __init__(self, tiles, name):
        self.tiles = tiles
        self.name = name
        self.i = 0

    def next(self):
        i = self.i
        self.i = (i + 1) % len(self.tiles)
        return self.tiles[i], (self.name, i)


def build():
    nc = bass.Bass("TRN2", target_bir_lowering=False)

    def din(name, shape):
        return nc.dram_tensor(name, list(shape), F32, kind="ExternalInput").ap()

    xT = din("xT", [1024, NT])
    cT = din("cT", [128, 8, 2])
    mod_w = din("mod_w", [4, 1024, 6144])
    mod_bT = din("mod_bT", [4, 128, 48])
    n1g = din("n1g", [4, 128, 8])
    n2g = din("n2g", [4, 128, 8])
    fgT = din("fgT", [128, 8])
    w_in = din("w_in", [2, 19, 1024, 128])
    w_out = din("w_out", [2, 1024, 1024])
    qg = din("qg", [2, 128, 1])
    kg = din("kg", [2, 128, 1])
    slg = din("slg", [2, 128, 1])
    lamv = din("lamv", [2, 128, 4, 64])
    cosT = din("cosT", [128, NT])
    sinT = din("sinT", [128, NT])
    rmat = din("rmat", [128, 128])
    ident = din("ident", [128, 128])
    bones = din("bones", [128, 128])
    s_are = din("s_are", [2, 2, 128, 32])
    s_aim = din("s_aim", [2, 2, 128, 32])
    s_ldt = din("s_ldt", [2, 2, 128, 32])
    s_bre = din("s_bre", [2, 2, 128, 32, 16])
    s_bim = din("s_bim", [2, 2, 128, 32, 16])
    s_cre = din("s_cre", [2, 2, 128, 32, 16])
    s_cim = din("s_cim", [2, 2, 128, 32, 16])
    s_d = din("s_d", [2, 128, 8])
    pmask = din("pmask", [128, 8])
    glu_a = din("glu_a", [2, 1024, 1024])
    glu_b = din("glu_b", [2, 1024, 1024])
    wr = din("wr", [4, 1024, 36])
    wrb = din("wrb", [4, 128, 36])
    w_gate = din("w_gate", [4, 32, 1024, 256])
    w_up = din("w_up", [4, 32, 1024, 256])
    w_down = din("w_down", [4, 32, 256, 1024])
    outT = nc.dram_tensor("outT", [1024, 2048], F32, kind="ExternalOutput").ap()
    hT = nc.dram_tensor("hT_scr", [1024, NT], F32, kind="Internal").ap()
    yscr = [nc.dram_tensor(f"y_scr{d}", [1024, NT], F32, kind="Internal").ap() for d in range(2)]

    hT3 = hT.rearrange("(k p) t -> p k t", p=128)
    xT3 = xT.rearrange("(k p) t -> p k t", p=128)
    outT3 = outT.rearrange("(k p) t -> p k t", p=128)
    y3 = [y.rearrange("(k p) t -> p k t", p=128) for y in yscr]

    with contextlib.ExitStack() as st:
        P = Prog(nc, st)

        def sb(name, shape, dt=F32):
            return st.enter_context(nc.sbuf_tensor(name, list(shape), dt))

        ps_tiles = [st.enter_context(nc.psum_tensor(f"ps{i}", [128, 512], F32)) for i in range(8)]

        def ps_get():
            i = P.psi
            P.psi = (i + 1) % 8
            return ps_tiles[i], ('ps', i)

        arena = sb("arena", [128, ARENA])
        xn = sb("xn", [128, 8, NT], BF16)
        ones_b = sb("ones_b", [128, 128], BF16)
        ona_b = sb("ona_b", [128, 128], BF16)
        onb_b = sb("onb_b", [128, 128], BF16)
        ones_f = sb("ones_f", [128, 128])
        ident_s = sb("ident_s", [128, 128])
        bones_s = sb("bones_s", [128, 128])
        rmat_s = sb("rmat_s", [128, 128])
        pm = sb("pm", [128, 8])
        cs = sb("cs", [128, 8, 2])
        fg_s = sb("fg_s", [128, 8])
        modT = sb("modT", [128, 48, 2])
        mbias = sb("mbias", [128, 48])
        gsc = sb("gsc", [128, 2, 8, 2])
        g12 = sb("g12", [128, 2, 8])
        apar = sb("apar", [128, 8])
        lam_s = sb("lam_s", [128, 4, 64])
        wrs = sb("wrs", [128, 8, 36])
        wrb_s = sb("wrb_s", [128, 36])
        dsk = sb("dsk", [128, 8])

        P.memset('pool', ones_b[:], 1.0, ['ones_b'])
        P.memset('pool', ones_f[:], 1.0, ['ones_f'])
        P.memset('pool', ona_b[:], 0.0, ['ona_b'])
        P.memset('pool', ona_b[:, 0:64], 1.0, ['ona_b'])
        P.memset('pool', onb_b[:], 0.0, ['onb_b'])
        P.memset('pool', onb_b[:, 64:128], 1.0, ['onb_b'])
        P.dma('sp', ident_s[:], ident, writes=['ident'])
        P.dma('sp', bones_s[:], bones, writes=['bones'])
        P.dma('sp', rmat_s[:], rmat, writes=['rmat'])
        P.dma('sp', pm[:], pmask, writes=['pm'])
        P.dma('sp', cs[:], cT, writes=['cs'])
        P.dma('sp', fg_s[:], fgT, writes=['fg'])
        P.act(cs[:], cs[:], AF.Silu, ['cs'], ['cs'])

        def mkrot(C, name, n, nel, shape_fn=None, bf=False):
            tiles = []
            for i in range(n):
                t = C.b(nel) if bf else C.f(nel)
                tiles.append(shape_fn(t) if shape_fn else t)
            return Rot(tiles, name)

        k8 = lambda t: t.rearrange("p (k t) -> p k t", k=8)

        C = Carver(arena)
        hb_rot = mkrot(C, "hb", 2, 4096, k8)
        for (t0, n) in BLKS:
            hb, hk = hb_rot.next()
            P.dma('sp', hb[:, :, 0:n], xT3[:, :, t0:t0 + n], writes=[hk])
            P.dma('sp', hT3[:, :, t0:t0 + n], hb[:, :, 0:n], reads=[hk], writes=[('h', t0)])
        P.barrier()

        def phase_mod(l):
            C = Carver(arena)
            mw_rot = mkrot(C, "mw", 2, 6144, lambda t: t.rearrange("p (k f) -> p k f", k=8))
            P.dma('sp', mbias[:], mod_bT[l], writes=['mbias'])
            P.dma('sp', g12[:, 0, :], n1g[l], writes=['g12'])
            P.dma('sp', g12[:, 1, :], n2g[l], writes=['g12'])
            pst, pk = ps_get()
            for fgp in range(8):
                mw, mk = mw_rot.next()
                P.dma('sp' if fgp % 2 == 0 else 'act', mw, mod_w[l][:, fgp * 768:(fgp + 1) * 768].rearrange("(k p) f -> p k f", p=128), writes=[mk])
                for fc in range(6):
                    f = fgp * 6 + fc
                    P.mm(pst[:, 2 * f:2 * f + 2],
                         [(mw[:, k, fc * 128:(fc + 1) * 128], cs[:, k, :]) for k in range(8)],
                         [mk, 'cs'], [pk])
            for j in range(2):
                P.tt('dve', modT[:, :, j], pst[:, 0:96].rearrange("p (f j) -> p f j", j=2)[:, :, j], mbias[:],
                     ALU.add, [pk, 'mbias'], ['modT'])
            for ni, base in ((0, 8), (1, 32)):
                for j in range(2):
                    P.ts1('dve', gsc[:, ni, :, j], modT[:, base:base + 8, j], 1.0, ALU.add, ['modT'], ['gsc'])
                    P.tt('dve', gsc[:, ni, :, j], gsc[:, ni, :, j], g12[:, ni, :], ALU.mult, ['gsc', 'g12'], ['gsc'])
            P.barrier()

        def do_norm(C, ni, shift_base, router=None, nbuf=2):
            hb_rot = mkrot(C, "nhb", nbuf, 4096, k8)
            sq_rot = mkrot(C, "nsq", nbuf, 4096, k8, bf=True)
            rs_rot = mkrot(C, "nrs", 2, 512)
            for bi, (t0, n) in enumerate(BLKS):
                j = 1 if bi == 0 else 0
                hb, hk = hb_rot.next()
                P.dma('sp', hb[:, :, 0:n], hT3[:, :, t0:t0 + n], reads=[('h', t0)], writes=[hk])
                sq, sk = sq_rot.next()
                P.act(sq[:, :, 0:n], hb[:, :, 0:n], AF.Square, [hk], [sk])
                pst, pk = ps_get()
                P.mm(pst[:, 0:n], [(ones_b[:], sq[:, k, 0:n]) for k in range(8)], ['ones_b', sk], [pk])
                rs, rk = rs_rot.next()
                P.act(rs[:, 0:n], pst[:, 0:n], AF.Sqrt, [pk], [rk], bias=EPS, scale=1.0 / 1024)
                P.recip(rs[:, 0:n], rs[:, 0:n], [rk], [rk])
                for k in range(8):
                    P.stt('dve', hb[:, k, 0:n], hb[:, k, 0:n], gsc[:, ni, k, j:j + 1], rs[:, 0:n],
                          ALU.mult, ALU.mult, [hk, 'gsc', rk], [hk])
                    if router is None:
                        P.act(xn[:, k, t0:t0 + n], hb[:, k, 0:n], AF.Identity, [hk, 'modT'], [('xn', t0)],
                              bias=modT[:, shift_base + k, j:j + 1], scale=1.0)
                    else:
                        P.act(hb[:, k, 0:n], hb[:, k, 0:n], AF.Identity, [hk, 'modT'], [hk],
                              bias=modT[:, shift_base + k, j:j + 1], scale=1.0)
                if router is not None:
                    P.act(xn[:, :, t0:t0 + n], hb[:, :, 0:n], AF.Copy, [hk], [('xn', t0)])
                    router(hb, hk, t0, n)

        def add_residual(hb_rot, t0, n, j, gate_base, get_ps):
            hb, hk = hb_rot.next()
            P.dma('sp', hb[:, :, 0:n], hT3[:, :, t0:t0 + n], reads=[('h', t0)], writes=[hk])
            for oc in range(8):
                src, srck = get_ps(oc)
                P.stt('dve', hb[:, oc, 0:n], src, modT[:, gate_base + oc, j:j + 1], hb[:, oc, 0:n],
                      ALU.mult, ALU.add, [hk, 'modT'] + srck, [hk])
            P.dma('sp', hT3[:, :, t0:t0 + n], hb[:, :, 0:n], reads=[hk], writes=[('h', t0)])

        xn_keys = [('xn', t0) for (t0, n) in BLKS]

        def phase_norm1():
            C = Carver(arena)
            do_norm(C, 0, 0)
            P.barrier()

        def phase_moe(l):
            C = Carver(arena)
            acc = k8(C.f(8 * NT))
            combT = C.f(NT)[0:32, :]
            rt_rot = mkrot(C, "rt", 2, 256)
            wg_rot = mkrot(C, "wg", 2, 2048, lambda t: t.rearrange("p (k f) -> p k f", k=8), bf=True)
            wu_rot = mkrot(C, "wu", 2, 2048, lambda t: t.rearrange("p (k f) -> p k f", k=8), bf=True)
            wd_rot = mkrot(C, "wd", 2, 2048, lambda t: t.rearrange("p (k f) -> p k f", k=2), bf=True)
            hid_rot = mkrot(C, "hid", 2, 1024, lambda t: t.rearrange("p (k f) -> p k f", k=2), bf=True)
            tmp_rot = mkrot(C, "mtmp", 3, 512)
            cme_rot = mkrot(C, "cme", 2, 512)
            P.dma('sp', wrs[:], wr[l].rearrange("(k p) f -> p k f", p=128), writes=['wrs'])
            P.dma('sp', wrb_s[:], wrb[l], writes=['wrb'])

            def router(x32, xk, t0, n):
                for tt_ in range(n // 128):
                    c0 = tt_ * 128
                    pst, pk = ps_get()
                    P.mm(pst[:, 0:36], [(x32[:, k, c0:c0 + 128], wrs[:, k, :]) for k in range(8)],
                         [xk, 'wrs'], [pk])
                    r, rk = rt_rot.next()
                    R = [rk]
                    lg = r[:, 0:36]
                    P.tt('dve', lg, pst[:, 0:36], wrb_s[:], ALU.add, [pk, 'wrb'], R)
                    gl = r[:, 0:4]
                    el = r[:, 4:36].rearrange("p (g e) -> p g e", g=4)
                    gmax = r[:, 40:41]
                    P.op('dve', lambda e, o=gmax, i=gl: e.reduce_max(o, i, AX.X), R, R)
                    goh = r[:, 44:48]
                    P.ts1('dve', goh, gl, gmax, ALU.is_ge, R, R)
                    gex = r[:, 48:52]
                    ngm = r[:, 41:42]
                    P.ts1('dve', ngm, gmax, -1.0, ALU.mult, R, R)
                    gsum = r[:, 42:43]
                    P.act(gex, gl, AF.Exp, R, R, bias=ngm, scale=1.0)
                    P.op('dve', lambda e, o=gsum, i=gex: e.reduce_sum(o, i, AX.X), R, R)
                    pgrp = r[:, 43:44]
                    P.recip(pgrp, gsum, R, R)
                    em = r[:, 64:96].rearrange("p (g e) -> p g e", g=4)
                    P.tt('dve', em, el, goh.unsqueeze(2).to_broadcast([128, 4, 8]), ALU.mult, R, R)
                    esel = r[:, 96:104]
                    P.op('dve', lambda e, o=esel, i=r[:, 64:96].rearrange("p (g e) -> p e g", g=4):
                         e.reduce_sum(o, i, AX.X), R, R)
                    m1 = r[:, 104:105]
                    P.op('dve', lambda e, o=m1, i=esel: e.reduce_max(o, i, AX.X), R, R)
                    mk1 = r[:, 112:120]
                    P.ts1('dve', mk1, esel, m1, ALU.is_ge, R, R)
                    es2 = r[:, 120:128]
                    P.stt('dve', es2, mk1, -1e30, esel, ALU.mult, ALU.add, R, R)
                    m2 = r[:, 105:106]
                    P.op('dve', lambda e, o=m2, i=es2: e.reduce_max(o, i, AX.X), R, R)
                    mk2 = r[:, 128:136]
                    P.ts1('dve', mk2, es2, m2, ALU.is_ge, R, R)
                    dm = r[:, 106:107]
                    P.tt('dve', dm, m1, m2, ALU.subtract, R, R)
                    P.act(dm, dm, AF.Exp, R, R)
                    P.ts1('dve', dm, dm, 1.0, ALU.add, R, R)
                    w2 = r[:, 107:108]
                    P.recip(w2, dm, R, R)
                    w1 = r[:, 108:109]
                    P.ts('dve', w1, w2, -1.0, 1.0, ALU.mult, ALU.add, R, R)
                    P.tt('dve', w1, w1, pgrp, ALU.mult, R, R)
                    P.tt('dve', w2, w2, pgrp, ALU.mult, R, R)
                    cl = r[:, 136:144]
                    P.ts1('dve', cl, mk1, w1, ALU.mult, R, R)
                    P.stt('dve', cl, mk2, w2, cl, ALU.mult, ALU.add, R, R)
                    comb = r[:, 160:192].rearrange("p (g e) -> p g e", g=4)
                    P.tt('dve', comb, goh.unsqueeze(2).to_broadcast([128, 4, 8]),
                         cl.unsqueeze(1).to_broadcast([128, 4, 8]), ALU.mult, R, R)
                    pt, ptk = ps_get()
                    P.tr(pt[0:32, 0:128], r[:, 160:192], ident_s[:], R + ['ident'], [ptk])
                    P.cp('dve', combT[:, t0 + c0:t0 + c0 + 128], pt[0:32, 0:128], [ptk], [('combT', t0)])

            C2 = Carver(arena)
            C2.o = C.o
            do_norm(C2, 1, 24, router=router, nbuf=1)
            hb_rot = Rot([k8(arena[:, C.o:C.o + 4096])], "nhb")

            def load(e):
                wg, wgk = wg_rot.next()
                wu, wuk = wu_rot.next()
                wd, wdk = wd_rot.next()
                P.dma('pool', wg, w_gate[l, e].rearrange("(p k) f -> p k f", k=8), writes=[wgk])
                P.dma('pool', wu, w_up[l, e].rearrange("(p k) f -> p k f", k=8), writes=[wuk])
                P.dma('pool', wd, w_down[l, e].rearrange("(k p) f -> p k f", p=128), writes=[wdk])
                return (wg, wgk, wu, wuk, wd, wdk)
            def front(e, bi, wts):
                wg, wgk, wu, wuk, wd, wdk = wts
                t0, n = BLKS[bi]
                pcb, pcbk = ps_get()
                P.mm(pcb[:, 0:n], [(ident_s[0:32, e:e + 1].to_broadcast([32, 128]), combT[:, t0:t0 + n])],
                     ['ident', ('combT', t0)], [pcbk])
                cbs, cbk = tmp_rot.next()
                P.act(cbs[:, 0:n], pcb[:, 0:n], AF.Copy, [pcbk], [cbk])
                hid, hidk = hid_rot.next()
                for hc in range(2):
                    pg, pgk = ps_get()
                    pu, puk = ps_get()
                    P.mm(pg[:, 0:n], [(wg[:, k, hc * 128:(hc + 1) * 128], xn[:, k, t0:t0 + n]) for k in range(8)],
                         [wgk, ('xn', t0)], [pgk])
                    P.mm(pu[:, 0:n], [(wu[:, k, hc * 128:(hc + 1) * 128], xn[:, k, t0:t0 + n]) for k in range(8)],
                         [wuk, ('xn', t0)], [puk])
                    sg, sgk = tmp_rot.next()
                    P.act(sg[:, 0:n], pg[:, 0:n], AF.Silu, [pgk], [sgk])
                    P.tt('dve', sg[:, 0:n], pu[:, 0:n], sg[:, 0:n], ALU.mult, [puk, sgk], [sgk])
                    P.tt('pool', hid[:, hc, 0:n], sg[:, 0:n], cbs[:, 0:n], ALU.mult, [sgk, cbk], [hidk])
                return (e, bi, hid, hidk, wd, wdk)

            def back(e, bi, hid, hidk, wd, wdk):
                t0, n = BLKS[bi]
                for dc in range(8):
                    po, pok = ps_get()
                    P.mm(po[:, 0:n], [(wd[:, hc, dc * 128:(dc + 1) * 128], hid[:, hc, 0:n]) for hc in range(2)],
                         [wdk, hidk], [pok])
                    if e == 0:
                        P.cp('dve', acc[:, dc, t0:t0 + n], po[:, 0:n], [pok], [('acc', t0)])
                    else:
                        P.tt('dve', acc[:, dc, t0:t0 + n], po[:, 0:n], acc[:, dc, t0:t0 + n], ALU.add,
                             [pok, ('acc', t0)], [('acc', t0)])

            W = {0: load(0), 1: load(1)}
            pend = None
            for e in range(32):
                for bi in range(len(BLKS)):
                    cur = front(e, bi, W[e])
                    if pend is not None:
                        back(*pend)
                    if bi == 0 and e >= 1 and e + 1 < 32:
                        W[e + 1] = load(e + 1)
                    pend = cur
            back(*pend)
            for bi, (t0, n) in enumerate(BLKS):
                j = 1 if bi == 0 else 0
                add_residual(hb_rot, t0, n, j, 40, lambda oc, t0=t0, n=n: (acc[:, oc, t0:t0 + n], [('acc', t0)]))
            P.barrier()

        def phase_attn(l):
            ia = l // 2
            lambda_init = 0.8 - 0.6 * math.exp(-0.3 * l)
            C = Carver(arena)
            merged = k8(C.b(8 * NT))
            qT = C.b(NT)
            kT = C.b(NT)
            VA = C.b(NT).rearrange("p (t c) -> p t c", c=128)
            VB = C.b(NT).rearrange("p (t c) -> p t c", c=128)
            wo = k8(C.b(8192))
            w3 = lambda t: t.rearrange("p (k f) -> p k f", k=8)
            wq_rot = mkrot(C, "wq", 2, 1024, w3, bf=True)
            wk_rot = mkrot(C, "wk", 2, 1024, w3, bf=True)
            wv_rot = mkrot(C, "wv", 2, 1024, w3, bf=True)
            tmp_rot = mkrot(C, "atmp", 4, 512)
            tmpb_rot = mkrot(C, "atmpb", 4, 512, bf=True)
            rs_rot = mkrot(C, "ars", 2, 512)
            cs_rot = mkrot(C, "acs", 2, 1024)
            hb_rot = mkrot(C, "ahb", 2, 4096, k8)
            import os as _os
            KA = int(_os.environ.get('KA', '7'))
            P.dma('sp', apar[:, 0:1], qg[ia], writes=['apar'])
            P.dma('sp', apar[:, 1:2], kg[ia], writes=['apar'])
            P.dma('sp', apar[:, 2:3], slg[ia], writes=['apar'])
            P.dma('sp', lam_s[:], lamv[ia], writes=['lam_s'])
            A = ['apar']
            if KA & 1:
              P.ts1('dve', apar[:, 2:3], apar[:, 2:3], 1.0 - lambda_init, ALU.mult, A, A)
            if KA & 1:
              P.tt('dve', lam_s[:, 0, :], lam_s[:, 0, :], lam_s[:, 1, :], ALU.mult, ['lam_s'], ['lam_s'])
            if KA & 1:
              P.tt('dve', lam_s[:, 2, :], lam_s[:, 2, :], lam_s[:, 3, :], ALU.mult, ['lam_s'], ['lam_s'])
            if KA & 1:
              P.op('dve', lambda e: e.reduce_sum(apar[:, 4:5], lam_s[:, 0, :], AX.X), ['lam_s'] + A, A)
            if KA & 1:
              P.op('dve', lambda e: e.reduce_sum(apar[:, 5:6], lam_s[:, 2, :], AX.X), ['lam_s'] + A, A)
            if KA & 1:
              P.act(apar[:, 4:6], apar[:, 4:6], AF.Exp, A, A)
            if KA & 1:
              P.tt('dve', apar[:, 3:4], apar[:, 5:6], apar[:, 4:5], ALU.subtract, A, A)
            if KA & 1:
              P.ts1('dve', apar[:, 3:4], apar[:, 3:4], -lambda_init, ALU.add, A, A)
            if KA & 2:
              P.dma('pool', wo, w_out[ia].rearrange("(k p) f -> p k f", p=128), writes=['wo'])

            def qk_post(pst, pk, is_gqa, gcol, dst, dk, t0, n):
                csb, csk = cs_rot.next()
                P.dma('sp', csb[:, 0:n], cosT[:, t0:t0 + n], writes=[csk])
                P.dma('sp', csb[:, 512:512 + n], sinT[:, t0:t0 + n], writes=[csk])
                if is_gqa:
                    sq, sk = tmp_rot.next()
                    P.act(sq[:, 0:n], pst[:, 0:n], AF.Square, [pk], [sk])
                    p2, p2k = ps_get()
                    P.mm(p2[:, 0:n], [(bones_s[:], sq[:, 0:n])], ['bones', sk], [p2k])
                    rs, rk = rs_rot.next()
                    P.act(rs[:, 0:n], p2[:, 0:n], AF.Sqrt, [p2k], [rk], bias=EPS, scale=1.0 / 64)
                    P.recip(rs[:, 0:n], rs[:, 0:n], [rk], [rk])
                    xs, xk = tmp_rot.next()
                    P.stt('dve', xs[:, 0:n], pst[:, 0:n], apar[:, gcol:gcol + 1], rs[:, 0:n], ALU.mult, ALU.mult,
                          [pk, rk, 'apar'], [xk])
                else:
                    xs, xk = tmp_rot.next()
                    P.act(xs[:, 0:n], pst[:, 0:n], AF.Copy, [pk], [xk])
                p3, p3k = ps_get()
                P.mm(p3[:, 0:n], [(rmat_s[:], xs[:, 0:n])], ['rmat', xk], [p3k])
                b_, bk = tmp_rot.next()
                P.tt('dve', b_[:, 0:n], p3[:, 0:n], csb[:, 512:512 + n], ALU.mult, [p3k, csk], [bk])
                P.tt('pool', xs[:, 0:n], xs[:, 0:n], csb[:, 0:n], ALU.mult, [xk, csk], [xk])
                P.tt('pool', dst[:, t0:t0 + n], xs[:, 0:n], b_[:, 0:n], ALU.add, [xk, bk], [dk])

            import os as _os
            KU = int(_os.environ.get('KU', '8'))
            KST = int(_os.environ.get('KST', '4'))
            for u in range(KU):
                gqa = u < 4
                if gqa:
                    qc, kc, vc, vw, vo = u, 4 + u // 2, 6, 64, (u // 2) * 64
                else:
                    h = u - 4
                    qc, kc, vc, vw, vo = 7 + h, 11 + h, 15 + h, 128, 0
                wq, wqk = wq_rot.next()
                wk, wkk = wk_rot.next()
                wv, wvk = wv_rot.next()
                if KA & 4:
                  P.dma('pool', wq, w_in[ia, qc].rearrange("(p k) f -> p k f", k=8), writes=[wqk])
                if KA & 4:
                  P.dma('pool', wk, w_in[ia, kc].rearrange("(p k) f -> p k f", k=8), writes=[wkk])
                if KA & 4:
                  P.dma('pool', wv, w_in[ia, vc].rearrange("(p k) f -> p k f", k=8), writes=[wvk])
                for (t0, n) in (BLKS if KST >= 1 else []):
                    pst, pk = ps_get()
                    P.mm(pst[:, 0:n], [(wq[:, k, :], xn[:, k, t0:t0 + n]) for k in range(8)], [wqk, ('xn', t0)], [pk])
                    if KST == 1 and int(_os.environ.get('KQ', '1')) == 0:
                        continue
                    qk_post(pst, pk, gqa, 0, qT, ('qT', t0), t0, n)
                    pst, pk = ps_get()
                    P.mm(pst[:, 0:n], [(wk[:, k, :], xn[:, k, t0:t0 + n]) for k in range(8)], [wkk, ('xn', t0)], [pk])
                    qk_post(pst, pk, gqa, 1, kT, ('kT', t0), t0, n)
                if KST < 2:
                    continue
                KV = int(_os.environ.get('KV', '15'))
                if gqa and (KV & 1):
                    P.memset('pool', VA[:, :, 64:128], 0.0, ['VA'])
                    P.memset('pool', VB[:, :, 0:64], 0.0, ['VB'])
                for tt_ in range(18 if (KV & 2) else 0):
                    pst, pk = ps_get()
                    P.mm(pst[:, 0:vw], [(xn[:, k, tt_ * 128:(tt_ + 1) * 128], wv[:, k, vo:vo + vw]) for k in range(8)],
                         [wvk] + xn_keys, [pk])
                    if gqa:
                        if KV & 4:
                            P.act(VA[:, tt_, 0:64], pst[:, 0:64], AF.Copy, [pk], ['VA'])
                        if KV & 8:
                            P.act(VB[:, tt_, 64:128], pst[:, 0:64], AF.Copy, [pk], ['VB'])
                    else:
                        P.act(VA[:, tt_, :], pst[:, 0:128], AF.Copy, [pk], ['VA'])
                kkeys = [('kT', t0) for (t0, n) in BLKS]
                if KST < 3:
                    continue
                for bi, (t0, n) in enumerate(BLKS):
                    nkt = 2 if bi == 0 else 18
                    O1, O1k = ps_tiles[0], ('ps', 0)
                    O2, O2k = ps_tiles[1], ('ps', 1)
                    S1, S1k = ps_tiles[2], ('ps', 2)
                    S2, S2k = ps_tiles[3], ('ps', 3)
                    def emit_s(kt):
                        ia_ = 4 + (2 * kt) % 4
                        sa, sak = ps_tiles[ia_], ('ps', ia_)
                        sbb, sbk = ps_tiles[ia_ + 1], ('ps', ia_ + 1)
                        ks = slice(kt * 128, (kt + 1) * 128)
                        P.mm(sa[:, 0:n], [(kT[0:64, ks], qT[0:64, t0:t0 + n])], kkeys + [('qT', t0)], [sak])
                        P.mm(sbb[:, 0:n], [(kT[64:128, ks], qT[64:128, t0:t0 + n])], kkeys + [('qT', t0)], [sbk])

                    def emit_pv(kt):
                        first, last = kt == 0, kt == nkt - 1
                        ia_ = 4 + (2 * kt) % 4
                        sa, sak = ps_tiles[ia_], ('ps', ia_)
                        sbb, sbk = ps_tiles[ia_ + 1], ('ps', ia_ + 1)
                        pa, pak = tmpb_rot.next()
                        pb, pbk = tmpb_rot.next()
                        P.act(pa[:, 0:n], sa[:, 0:n], AF.Exp, [sak], [pak], scale=0.125)
                        P.act(pb[:, 0:n], sbb[:, 0:n], AF.Exp, [sbk], [pbk], scale=0.125)
                        if gqa:
                            P.mm1(O1[:, 0:n], VA[:, kt, :], pa[:, 0:n], first, False, ['VA', pak], [O1k])
                            P.mm1(O1[:, 0:n], VB[:, kt, :], pb[:, 0:n], False, last, ['VB', pbk], [O1k])
                            P.mm1(S1[:, 0:n], ona_b[:], pa[:, 0:n], first, False, ['ona_b', pak], [S1k])
                            P.mm1(S1[:, 0:n], onb_b[:], pb[:, 0:n], False, last, ['onb_b', pbk], [S1k])
                        else:
                            P.mm1(O1[:, 0:n], VA[:, kt, :], pa[:, 0:n], first, last, ['VA', pak], [O1k])
                            P.mm1(O2[:, 0:n], VA[:, kt, :], pb[:, 0:n], first, last, ['VA', pbk], [O2k])
                            P.mm1(S1[:, 0:n], ones_b[:], pa[:, 0:n], first, last, ['ones_b', pak], [S1k])
                            P.mm1(S2[:, 0:n], ones_b[:], pb[:, 0:n], first, last, ['ones_b', pbk], [S2k])

                    emit_s(0)
                    for kt in range(nkt):
                        if kt + 1 < nkt:
                            emit_s(kt + 1)
                        emit_pv(kt)
                    r1, r1k = tmp_rot.next()
                    P.recip(r1[:, 0:n], S1[:, 0:n], [S1k], [r1k])
                    if gqa:
                        P.tt('dve', merged[:, u, t0:t0 + n], O1[:, 0:n], r1[:, 0:n], ALU.mult, [O1k, r1k], [('mg', t0)])
                    else:
                        r2, r2k = tmp_rot.next()
                        P.recip(r2[:, 0:n], S2[:, 0:n], [S2k], [r2k])
                        P.tt('dve', r1[:, 0:n], O1[:, 0:n], r1[:, 0:n], ALU.mult, [O1k, r1k], [r1k])
                        P.tt('dve', r2[:, 0:n], O2[:, 0:n], r2[:, 0:n], ALU.mult, [O2k, r2k], [r2k])
                        P.stt('dve', r1[:, 0:n], r2[:, 0:n], apar[:, 3:4], r1[:, 0:n], ALU.mult, ALU.add,
                              [r1k, r2k, 'apar'], [r1k])
                        sq, sk = tmpb_rot.next()
                        P.act(sq[:, 0:n], r1[:, 0:n], AF.Square, [r1k], [sk])
                        p2, p2k = ps_tiles[4], ('ps', 4)
                        P.mm(p2[:, 0:n], [(ones_b[:], sq[:, 0:n])], ['ones_b', sk], [p2k])
                        P.act(r2[:, 0:n], p2[:, 0:n], AF.Sqrt, [p2k], [r2k], bias=EPS, scale=1.0 / 128)
                        P.recip(r2[:, 0:n], r2[:, 0:n], [r2k], [r2k])
                        P.stt('dve', merged[:, u, t0:t0 + n], r1[:, 0:n], apar[:, 2:3], r2[:, 0:n], ALU.mult, ALU.mult,
                              [r1k, r2k, 'apar'], [('mg', t0)])
            for bi, (t0, n) in enumerate(BLKS if KST >= 4 else []):
                j = 1 if bi == 0 else 0

                def get_ps(oc, t0=t0, n=n):
                    po, pok = ps_get()
                    P.mm(po[:, 0:n], [(wo[:, k, oc * 128:(oc + 1) * 128], merged[:, k, t0:t0 + n]) for k in range(8)],
                         ['wo', ('mg', t0)], [pok])
                    return po[:, 0:n], [pok]
                add_residual(hb_rot, t0, n, j, 16, get_ps)
            P.barrier()

        def phase_ssm(l):
            i_s = l // 2
            C = Carver(arena)
            TT = 256
            prm = C.f(32 * 20).rearrange("p (a b) -> p a b", b=32)
            c16 = lambda t: t.rearrange("p (a c) -> p a c", c=16)
            st4 = lambda t: t.rearrange("p (s t) -> p s t", t=TT)
            Wp = [C.f(64).rearrange("p (a b) -> p a b", b=32) for _ in range(2)]
            RRp = [C.f(32) for _ in range(2)]
            Bpad = [C.b(64 * 128).rearrange("p (s c) -> p s c", c=128) for _ in range(2)]
            Cpad = [C.b(64 * 128).rearrange("p (s c) -> p s c", c=128) for _ in range(2)]
            o_work = C.o
            bre, bim, cre, cim, bbr, bbi = [c16(C.f(512)) for _ in range(6)]
            ych = C.f(128)
            K = ['ssmp']
            P.dma('sp', dsk[:], s_d[i_s], writes=['dsk'])

            for d in range(2):
                are, aim, ldt = prm[:, 0, :], prm[:, 1, :], prm[:, 2, :]
                P.dma('sp', are, s_are[i_s, d], writes=K)
                P.dma('sp', aim, s_aim[i_s, d], writes=K)
                P.dma('sp', ldt, s_ldt[i_s, d], writes=K)
                P.dma('sp', bre, s_bre[i_s, d], writes=K)
                P.dma('sp', bim, s_bim[i_s, d], writes=K)
                P.dma('sp', cre, s_cre[i_s, d], writes=K)
                P.dma('sp', cim, s_cim[i_s, d], writes=K)
                lre, dt, zr, zi, t1, t2, nr, den, cr_, ci_, rr, wr, wi, u1, u2 = [prm[:, 3 + q, :] for q in range(15)]
                P.ts1('dve', lre, are, -1e-4, ALU.min, K, K)
                P.act(dt, ldt, AF.Exp, K, K)
                P.tt('dve', t1, lre, dt, ALU.mult, K, K)
                P.tt('dve', t2, aim, dt, ALU.mult, K, K)
                P.act(rr, t1, AF.Exp, K, K)
                P.act(zr, t1, AF.Exp, K, K, scale=1.0 / 16)
                P.act(zi, t2, AF.Sin, K, K, scale=1.0 / 16)
                P.ts('dve', t2, t2, 1.0 / 16, math.pi / 2, ALU.mult, ALU.add, K, K)
                P.act(t2, t2, AF.Sin, K, K)
                P.cp('dve', wi, zi, K, K)
                P.cp('dve', wr, t2, K, K)
                P.tt('dve', zi, zi, zr, ALU.mult, K, K)
                P.tt('dve', zr, t2, zr, ALU.mult, K, K)
                for _ in range(4):
                    P.tt('dve', t1, zr, zr, ALU.mult, K, K)
                    P.tt('dve', t2, zi, zi, ALU.mult, K, K)
                    P.tt('dve', zi, zr, zi, ALU.mult, K, K)
                    P.ts1('dve', zi, zi, 2.0, ALU.mult, K, K)
                    P.tt('dve', zr, t1, t2, ALU.subtract, K, K)
                def wsq():
                    P.tt('dve', u1, wr, wr, ALU.mult, K, K)
                    P.tt('dve', u2, wi, wi, ALU.mult, K, K)
                    P.tt('dve', wi, wr, wi, ALU.mult, K, K)
                    P.ts1('dve', wi, wi, 2.0, ALU.mult, K, K)
                    P.tt('dve', wr, u1, u2, ALU.subtract, K, K)
                for _ in range(4):
                    wsq()
                P.cp('dve', Wp[d][:, 0, :], wr, K, K)
                P.cp('dve', Wp[d][:, 1, :], wi, K, K)
                P.cp('dve', RRp[d], rr, K, K)
                P.ts1('dve', nr, zr, -1.0, ALU.add, K, K)
                P.tt('dve', den, lre, lre, ALU.mult, K, K)
                P.tt('dve', t1, aim, aim, ALU.mult, K, K)
                P.tt('dve', den, den, t1, ALU.add, K, K)
                P.recip(den, den, K, K)
                P.tt('dve', t1, nr, lre, ALU.mult, K, K)
                P.tt('dve', t2, zi, aim, ALU.mult, K, K)
                P.tt('dve', cr_, t1, t2, ALU.add, K, K)
                P.tt('dve', cr_, cr_, den, ALU.mult, K, K)
                P.tt('dve', t1, zi, lre, ALU.mult, K, K)
                P.tt('dve', t2, nr, aim, ALU.mult, K, K)
                P.tt('dve', ci_, t1, t2, ALU.subtract, K, K)
                P.tt('dve', ci_, ci_, den, ALU.mult, K, K)
                crb = cr_.unsqueeze(2).to_broadcast([128, 32, 16])
                cib = ci_.unsqueeze(2).to_broadcast([128, 32, 16])
                P.tt('dve', bbr, bre, crb, ALU.mult, K, K)
                P.tt('dve', bbi, bim, cib, ALU.mult, K, K)
                P.tt('dve', bbr, bbr, bbi, ALU.subtract, K, K)
                P.tt('dve', bbi, bre, cib, ALU.mult, K, K)
                P.tt('dve', bre, bim, crb, ALU.mult, K, K)
                P.tt('dve', bbi, bbi, bre, ALU.add, K, K)
                for dc in range(8):
                    for ri, src in ((0, bbr), (1, bbi)):
                        yv = ych.rearrange("p (q g c) -> p q g c", q=4, g=2)
                        for g2 in range(2):
                            P.ts1('dve', yv[:, :, g2, :], src[:, dc * 4:(dc + 1) * 4, :], pm[:, g2:g2 + 1],
                                  ALU.mult, K + ['pm', 'ych'], ['ych'])
                        pt, ptk = ps_get()
                        P.tr(pt[:, 0:128], ych, ident_s[:], ['ych', 'ident'], [ptk])
                        for q in range(4):
                            P.ts1('dve', Bpad[d][:, dc * 8 + q * 2 + ri, :], pt[:, 0:128], pm[:, 4 + q:5 + q],
                                  ALU.mult, [ptk, 'pm'], [('Bpad', d)])
                P.memset('pool', Cpad[d], 0.0, [('Cpad', d)])
                for pr in range(32):
                    dc, q = pr // 4, pr % 4
                    for ri, src, mo in ((0, cre, 0), (1, cim, 2)):
                        cv = Cpad[d][:, dc * 8 + q * 2 + ri, :].rearrange("p (q g c) -> p q g c", q=4, g=2)
                        for g2 in range(2):
                            P.ts1('pool', cv[:, q, g2, :], src[:, pr, :], pm[:, mo + g2:mo + g2 + 1],
                                  ALU.mult, K + ['pm'], [('Cpad', d)])

            def blocks_for(d):
                if d == 0:
                    return [(b * TT, False) for b in range(NT // TT)]
                return [(0, True)] + [(b * TT, True) for b in range(NT // TT - 1, 0, -1)]
            seqs = [blocks_for(0), blocks_for(1)]
            P.barrier()
            C.o = o_work
            COSa = st4(C.f(8 * TT))
            SINa = st4(C.f(8 * TT))
            RTa = st4(C.f(8 * TT))
            COSb = [COSa[:, d_ * 4:(d_ + 1) * 4, :] for d_ in range(2)]
            SINb = [SINa[:, d_ * 4:(d_ + 1) * 4, :] for d_ in range(2)]
            RTb = [RTa[:, d_ * 4:(d_ + 1) * 4, :] for d_ in range(2)]
            R2b = [C.f(8) for _ in range(2)]
            pw = C.f(32).rearrange("p (a b) -> p a b", b=8)
            BUs = [st4(C.f(8 * TT)) for _ in range(2)]
            Qin = [st4(C.f(8 * TT)) for _ in range(2)]
            tm = [st4(C.f(4 * TT)) for _ in range(4)]
            tmk = [('stm', i) for i in range(4)]
            Sb = [st4(C.b(8 * TT)) for _ in range(2)]
            spv = [C.f(8) for _ in range(2)]
            fx = [C.f(8) for _ in range(2)]
            yd = [C.f(TT) for _ in range(2)]

            def build_tab(dc):
                K2 = [('tab', 0), ('tab', 1)]
                p4 = slice(dc * 4, dc * 4 + 4)
                wr_, wi_, u1_, u2_ = [pw[:, i, :] for i in range(4)]
                for d_ in range(2):
                    P.cp('dve', wr_[:, d_ * 4:(d_ + 1) * 4], Wp[d_][:, 0, p4], [], K2)
                    P.cp('dve', wi_[:, d_ * 4:(d_ + 1) * 4], Wp[d_][:, 1, p4], [], K2)
                P.cp('dve', COSa[:, :, 0], wr_, K2, K2)
                P.cp('dve', SINa[:, :, 0], wi_, K2, K2)
                ta = tm[0].rearrange("p s t -> p (s t)")[:, 0:8 * (TT // 2)].rearrange("p (s t) -> p s t", t=TT // 2)
                tb = tm[1].rearrange("p s t -> p (s t)")[:, 0:8 * (TT // 2)].rearrange("p (s t) -> p s t", t=TT // 2)
                kk = 1
                while kk < TT:
                    cb = wr_.unsqueeze(2).to_broadcast([128, 8, kk])
                    sb_ = wi_.unsqueeze(2).to_broadcast([128, 8, kk])
                    c0 = COSa[:, :, 0:kk]
                    s0 = SINa[:, :, 0:kk]
                    a_ = ta[:, :, 0:kk]
                    b_ = tb[:, :, 0:kk]
                    P.tt('dve', a_, c0, cb, ALU.mult, K2, [tmk[0]])
                    P.tt('dve', b_, s0, sb_, ALU.mult, K2, [tmk[1]])
                    P.tt('dve', COSa[:, :, kk:2 * kk], a_, b_, ALU.subtract, [tmk[0], tmk[1]], K2)
                    P.tt('dve', a_, s0, cb, ALU.mult, K2, [tmk[0]])
                    P.tt('dve', b_, c0, sb_, ALU.mult, K2, [tmk[1]])
                    P.tt('dve', SINa[:, :, kk:2 * kk], a_, b_, ALU.add, [tmk[0], tmk[1]], K2)
                    kk *= 2
                    if kk < TT:
                        P.tt('dve', u1_, wr_, wr_, ALU.mult, K2, K2)
                        P.tt('dve', u2_, wi_, wi_, ALU.mult, K2, K2)
                        P.tt('dve', wi_, wr_, wi_, ALU.mult, K2, K2)
                        P.ts1('dve', wi_, wi_, 2.0, ALU.mult, K2, K2)
                        P.tt('dve', wr_, u1_, u2_, ALU.subtract, K2, K2)
                for d_ in range(2):
                    P.cp('dve', RTb[d_], RRp[d_][:, p4].unsqueeze(2).to_broadcast([128, 4, TT]), [], K2)
                    P.cp('dve', R2b[d_][:, 0:4], RRp[d_][:, p4], [], K2)
                    P.cp('dve', R2b[d_][:, 4:8], RRp[d_][:, p4], [], K2)
                P.memset('dve', RTa[:, :, 0:1], 0.0, K2)

            for dc in range(8):
                build_tab(dc)
                for bidx in range(NT // TT):
                    def stage_ab(d):
                        t0, rev = seqs[d][bidx]
                        kT_ = ('tab', d)
                        kBr, kBi, kQr, kQi = ('BUr', d), ('BUi', d), ('Qr', d), ('Qi', d)
                        for ri in range(2):
                            for qh in range(2):
                                pst, pk = ps_get()
                                for q2 in range(2):
                                    q = qh * 2 + q2
                                    P.mm1(pst[:, q2 * TT:(q2 + 1) * TT], Bpad[d][:, dc * 8 + q * 2 + ri, :],
                                          xn[:, dc, t0:t0 + TT], True, True, xn_keys, [pk])
                                src = pst[:, 0:512].rearrange("p (s t) -> p s t", t=TT)
                                if rev:
                                    src = src[:, :, ::-1]
                                ls = ri * 4 + qh * 2
                                P.act(BUs[d][:, ls:ls + 2, :], src, AF.Copy, [pk], [kBr if ri == 0 else kBi])
                        Bre, Bim = BUs[d][:, 0:4, :], BUs[d][:, 4:8, :]
                        Qr, Qi = Qin[d][:, 0:4, :], Qin[d][:, 4:8, :]
                        P.tt('pool', tm[2 + d], Bre, SINb[d], ALU.mult, [kBr, kT_], [tmk[2 + d]])
                        P.tt('dve', Qi, Bim, COSb[d], ALU.mult, [kBi, kT_], [kQi])
                        P.tt('pool', Qi, Qi, tm[2 + d], ALU.subtract, [kQi, tmk[2 + d]], [kQi])
                        P.tt('dve', tm[0], Bre, COSb[d], ALU.mult, [kBr, kT_], [tmk[0]])
                        P.tt('dve', tm[1], Bim, SINb[d], ALU.mult, [kBi, kT_], [tmk[1]])
                        P.tt('dve', Qr, tm[0], tm[1], ALU.add, [tmk[0], tmk[1]], [kQr])

                    def stage_c(d):
                        kT_ = ('tab', d)
                        kQr, kQi = ('Qr', d), ('Qi', d)
                        if bidx > 0:
                            P.tt('dve', fx[d], spv[d], R2b[d], ALU.mult, [('sp', d), kT_], [('fx', d)])
                            q0 = Qin[d][:, :, 0]
                            P.tt('dve', q0, q0, fx[d], ALU.add, [kQr, kQi, ('fx', d)], [kQr, kQi])
                        rt2 = RTb[d].rearrange("p s t -> p (s t)")
                        for h, kq in ((0, kQr), (1, kQi)):
                            v = Qin[d][:, h * 4:(h + 1) * 4, :].rearrange("p s t -> p (s t)")
                            P.op('dve', lambda e, v=v, rt2=rt2: e.tensor_tensor_scan(v, rt2, v, 0.0, ALU.mult, ALU.add),
                                 [kq, kT_], [kq])

                    def stage_d(d):
                        kT_ = ('tab', d)
                        kBr, kBi, kQr, kQi = ('BUr', d), ('BUi', d), ('Qr', d), ('Qi', d)
                        Bre, Bim = BUs[d][:, 0:4, :], BUs[d][:, 4:8, :]
                        Qr, Qi = Qin[d][:, 0:4, :], Qin[d][:, 4:8, :]
                        P.tt('pool', tm[2 + d], Qr, SINb[d], ALU.mult, [kQr, kT_], [tmk[2 + d]])
                        P.tt('dve', Bim, Qi, COSb[d], ALU.mult, [kQi, kT_], [kBi])
                        P.tt('pool', Bim, Bim, tm[2 + d], ALU.add, [kBi, tmk[2 + d]], [kBi])
                        P.tt('dve', tm[0], Qr, COSb[d], ALU.mult, [kQr, kT_], [tmk[0]])
                        P.tt('dve', tm[1], Qi, SINb[d], ALU.mult, [kQi, kT_], [tmk[1]])
                        P.tt('dve', Bre, tm[0], tm[1], ALU.subtract, [tmk[0], tmk[1]], [kBr])
                        P.cp('pool', spv[d], BUs[d][:, :, TT - 1], [kBr, kBi], [('sp', d)])

                    def stage_ef(d):
                        t0, rev = seqs[d][bidx]
                        kBr, kBi = ('BUr', d), ('BUi', d)
                        src = BUs[d][:, :, ::-1] if rev else BUs[d]
                        P.act(Sb[d], src, AF.Copy, [kBr, kBi], [('Sb', d)])
                        pst, pk = ps_get()
                        prs = []
                        for q in range(4):
                            for ri in range(2):
                                prs.append((Cpad[d][:, dc * 8 + q * 2 + ri, :], Sb[d][:, ri * 4 + q, :]))
                        P.mm(pst[:, 0:TT], prs, [('Sb', d)], [pk])
                        P.act(yd[d], pst[:, 0:TT], AF.Copy, [pk], [('yd', d)])
                        P.dma('sp', y3[d][:, dc, t0:t0 + TT], yd[d], reads=[('yd', d)], writes=[('y', d)])

                    stage_ab(0)
                    stage_ab(1)
                    stage_c(0)
                    stage_c(1)
                    stage_d(0)
                    stage_d(1)
                    stage_ef(0)
                    stage_ef(1)
            P.barrier()

            C = Carver(arena)
            wa = k8(C.b(8192))
            wb = k8(C.b(8192))
            yf_rot = mkrot(C, "yfb", 1, 4096, k8)
            yb_rot = mkrot(C, "ybb", 1, 4096, k8)
            vb_rot = mkrot(C, "vbf", 1, 4096, k8, bf=True)
            hb_rot = mkrot(C, "shb", 2, 4096, k8)
            tmp_rot = mkrot(C, "stmpg", 3, 512)
            P.dma('pool', wa, glu_a[i_s].rearrange("(k p) f -> p k f", p=128), writes=['wa'])
            P.dma('pool', wb, glu_b[i_s].rearrange("(k p) f -> p k f", p=128), writes=['wb'])
            for bi, (t0, n) in enumerate(BLKS):
                j = 1 if bi == 0 else 0
                yfb, yfk = yf_rot.next()
                ybb, ybk = yb_rot.next()
                vbf, vbk = vb_rot.next()
                P.dma('sp', yfb[:, :, 0:n], y3[0][:, :, t0:t0 + n], writes=[yfk])
                P.dma('sp', ybb[:, :, 0:n], y3[1][:, :, t0:t0 + n], writes=[ybk])
                P.tt('dve', yfb[:, :, 0:n], yfb[:, :, 0:n], ybb[:, :, 0:n], ALU.add, [yfk, ybk], [yfk])
                for k in range(8):
                    P.stt('dve', yfb[:, k, 0:n], xn[:, k, t0:t0 + n], dsk[:, k:k + 1], yfb[:, k, 0:n], ALU.mult, ALU.add,
                          [yfk, 'dsk', ('xn', t0)], [yfk])
                P.act(vbf[:, :, 0:n], yfb[:, :, 0:n], AF.Gelu, [yfk], [vbk])

                def get_ps(oc, t0=t0, n=n, vbf=vbf, vbk=vbk):
                    pa_, pak_ = ps_get()
                    pb_, pbk_ = ps_get()
                    P.mm(pa_[:, 0:n], [(wa[:, k, oc * 128:(oc + 1) * 128], vbf[:, k, 0:n]) for k in range(8)],
                         ['wa', vbk], [pak_])
                    P.mm(pb_[:, 0:n], [(wb[:, k, oc * 128:(oc + 1) * 128], vbf[:, k, 0:n]) for k in range(8)],
                         ['wb', vbk], [pbk_])
                    sg, sgk = tmp_rot.next()
                    P.act(sg[:, 0:n], pb_[:, 0:n], AF.Sigmoid, [pbk_], [sgk])
                    P.tt('dve', sg[:, 0:n], pa_[:, 0:n], sg[:, 0:n], ALU.mult, [pak_, sgk], [sgk])
                    return sg[:, 0:n], [sgk]
                add_residual(hb_rot, t0, n, j, 16, get_ps)
            P.barrier()

        import os as _os
        kph = int(_os.environ.get('KPH', '4'))
        for l in range(NLAYERS):
            phase_mod(l)
            if kph >= 2:
                phase_norm1()
            if kph >= 3:
                if l % 2 == 0:
                    phase_attn(l)
                else:
                    phase_ssm(l)
            if kph >= 4:
                phase_moe(l)

        C = Carver(arena)
        hb_rot = mkrot(C, "fhb", 2, 4096, k8)
        sq_rot = mkrot(C, "fsq", 2, 4096, k8, bf=True)
        rs_rot = mkrot(C, "frs", 2, 512)
        for bi, (t0, n) in enumerate(BLKS):
            if bi == 0:
                continue
            hb, hk = hb_rot.next()
            P.dma('sp', hb[:, :, 0:n], hT3[:, :, t0:t0 + n], reads=[('h', t0)], writes=[hk])
            sq, sk = sq_rot.next()
            P.act(sq[:, :, 0:n], hb[:, :, 0:n], AF.Square, [hk], [sk])
            pst, pk = ps_get()
            P.mm(pst[:, 0:n], [(ones_b[:], sq[:, k, 0:n]) for k in range(8)], ['ones_b', sk], [pk])
            rs, rk = rs_rot.next()
            P.act(rs[:, 0:n], pst[:, 0:n], AF.Sqrt, [pk], [rk], bias=EPS, scale=1.0 / 1024)
            P.recip(rs[:, 0:n], rs[:, 0:n], [rk], [rk])
            for k in range(8):
                P.stt('dve', hb[:, k, 0:n], hb[:, k, 0:n], fg_s[:, k:k + 1], rs[:, 0:n], ALU.mult, ALU.mult,
                      [hk, 'fg', rk], [hk])
            P.dma('sp', outT3[:, :, t0 - NCX:t0 - NCX + n], hb[:, :, 0:n], reads=[hk], writes=[('out', t0)])
        P.finish('sp')
        P.emit()
    return nc


def _pk(v):
    return np.ascontiguousarray(v.reshape(8, 128).T)


def _host_consts():
    f32 = np.float32
    half = 32
    inv = (1.0 / (10000.0 ** (np.arange(0, half, 2, dtype=f32) / half))).astype(f32)
    t = np.arange(2048)
    rows = (t // 64).astype(f32)
    cols = (t % 64).astype(f32)
    ang_r = rows[:, None] * inv
    ang_c = cols[:, None] * inv
    ang = np.concatenate([ang_r, ang_r, ang_c, ang_c], axis=-1).astype(f32)
    cos = np.cos(ang).astype(f32)
    sin = np.sin(ang).astype(f32)
    cosT = np.ones((128, NT), f32)
    sinT = np.zeros((128, NT), f32)
    cosT[:, NCX:] = np.tile(cos.T, (2, 1))
    sinT[:, NCX:] = np.tile(sin.T, (2, 1))
    R = np.zeros((64, 64), f32)
    for s in (0, 32):
        for i in range(16):
            R[s + i, s + 16 + i] = -1.0
            R[s + 16 + i, s + i] = 1.0
    Rb = np.zeros((128, 128), f32)
    Rb[0:64, 0:64] = R
    Rb[64:128, 64:128] = R
    rmat = np.ascontiguousarray(Rb.T)
    ident = np.eye(128, dtype=f32)
    bones = np.zeros((128, 128), f32)
    bones[0:64, 0:64] = 1.0
    bones[64:128, 64:128] = 1.0
    pmask = np.zeros((128, 8), f32)
    pmask[0:64, 0] = 1.0
    pmask[64:128, 1] = 1.0
    pmask[0:64, 2] = -1.0
    pmask[64:128, 3] = -1.0
    for q in range(4):
        pmask[q * 32:(q + 1) * 32, 4 + q] = 1.0
    return dict(cosT=cosT, sinT=sinT, rmat=rmat, ident=ident, bones=bones, pmask=pmask)


_NC_CACHE = {}


def kernel(x, c, ctx, c_ctx, mod_w, mod_b, norm1_g, norm2_g, final_g,
           attn_w_in, attn_w_out, attn_q_norm_g, attn_k_norm_g,
           diff_lambda_q1, diff_lambda_k1, diff_lambda_q2, diff_lambda_k2, diff_subln_g,
           ssm_a_re, ssm_a_im, ssm_log_dt, ssm_b_re, ssm_b_im, ssm_c_re, ssm_c_im, ssm_d,
           ssm_glu_w_a, ssm_glu_w_b,
           moe_group_w, moe_group_b, moe_router_w, moe_router_b, moe_w_gate, moe_w_up, moe_w_down):
    f32 = np.float32
    A = lambda a: np.ascontiguousarray(np.asarray(a, dtype=f32))
    x, c, ctx, c_ctx = A(x), A(c), A(ctx), A(c_ctx)
    shared = _host_consts()
    shared["mod_w"] = A(mod_w)
    shared["mod_bT"] = A(np.asarray(mod_b).reshape(4, 48, 128).transpose(0, 2, 1))
    shared["n1g"] = A(np.asarray(norm1_g).reshape(4, 8, 128).transpose(0, 2, 1))
    shared["n2g"] = A(np.asarray(norm2_g).reshape(4, 8, 128).transpose(0, 2, 1))
    shared["fgT"] = _pk(np.asarray(final_g, dtype=f32))
    wi = np.asarray(attn_w_in, dtype=f32)
    qa, ka, va, qb, kb, vb = (wi[:, :, 0:512], wi[:, :, 512:640], wi[:, :, 640:768], wi[:, :, 768:1280],
                              wi[:, :, 1280:1792], wi[:, :, 1792:2304])
    wcat = np.concatenate([qa, ka[:, :, 0:64], ka[:, :, 0:64], ka[:, :, 64:128], ka[:, :, 64:128],
                           va, qb, kb, vb], axis=2)
    shared["w_in"] = A(wcat.reshape(2, 8, 128, 19, 128).transpose(0, 3, 2, 1, 4).reshape(2, 19, 1024, 128))
    shared["w_out"] = A(attn_w_out)
    shared["qg"] = A(np.tile(np.asarray(attn_q_norm_g, dtype=f32), (1, 2))[:, :, None])
    shared["kg"] = A(np.tile(np.asarray(attn_k_norm_g, dtype=f32), (1, 2))[:, :, None])
    shared["slg"] = A(np.asarray(diff_subln_g, dtype=f32)[:, :, None])
    lv = np.stack([np.asarray(a, dtype=f32) for a in (diff_lambda_q1, diff_lambda_k1, diff_lambda_q2, diff_lambda_k2)], axis=1)
    shared["lamv"] = A(np.broadcast_to(lv[:, None, :, :], (2, 128, 4, 64)))

    def LL(a):
        a = np.asarray(a, dtype=f32).reshape(2, 2, 32, 2, 64)
        return A(a.transpose(0, 1, 3, 4, 2).reshape(2, 2, 128, 32))
    shared["s_are"] = LL(ssm_a_re)
    shared["s_aim"] = LL(ssm_a_im)
    shared["s_ldt"] = LL(np.broadcast_to(np.asarray(ssm_log_dt, dtype=f32)[:, :, :, None], (2, 2, 64, 64)))

    def LB(a):
        a = np.asarray(a, dtype=f32).reshape(2, 2, 32, 2, 64, 16)
        return A(a.transpose(0, 1, 3, 4, 2, 5).reshape(2, 2, 128, 32, 16))

    def LC(a):
        a = np.asarray(a, dtype=f32).reshape(2, 2, 32, 2, 16, 64)
        return A(a.transpose(0, 1, 3, 5, 2, 4).reshape(2, 2, 128, 32, 16))
    shared["s_bre"] = LB(ssm_b_re)
    shared["s_bim"] = LB(ssm_b_im)
    shared["s_cre"] = LC(ssm_c_re)
    shared["s_cim"] = LC(ssm_c_im)
    shared["s_d"] = A(np.asarray(ssm_d, dtype=f32).reshape(2, 8, 128).transpose(0, 2, 1))
    shared["glu_a"] = A(ssm_glu_w_a)
    shared["glu_b"] = A(ssm_glu_w_b)
    gw = np.asarray(moe_group_w, dtype=f32)
    rw = np.asarray(moe_router_w, dtype=f32)
    shared["wr"] = A(np.concatenate([gw, rw.transpose(0, 2, 1, 3).reshape(4, 1024, 32)], axis=2))
    rb = np.concatenate([np.asarray(moe_group_b, dtype=f32), np.asarray(moe_router_b, dtype=f32).reshape(4, 32)], axis=1)
    shared["wrb"] = A(np.broadcast_to(rb[:, None, :], (4, 128, 36)))
    pk8 = lambda w: A(np.asarray(w, dtype=f32).reshape(4, 32, 8, 128, 256).transpose(0, 1, 3, 2, 4).reshape(4, 32, 1024, 256))
    shared["w_gate"] = pk8(moe_w_gate)
    shared["w_up"] = pk8(moe_w_up)
    shared["w_down"] = A(moe_w_down)

    if "nc" not in _NC_CACHE:
        _NC_CACHE["nc"] = build()
    nc = _NC_CACHE["nc"]
    in_maps = []
    import os as _os
    ncores = int(_os.environ.get('KCORES', '8'))
    for b in range(ncores):
        m = dict(shared)
        m["xT"] = A(np.concatenate([ctx[b], x[b]], axis=0).T)
        cc = np.stack([c[b], c_ctx], axis=1)
        m["cT"] = A(cc.reshape(8, 128, 2).transpose(1, 0, 2))
        in_maps.append(m)
    res = run_bass_kernel_spmd(nc, in_maps, core_ids=list(range(ncores)))
    out = np.stack([np.ascontiguousarray(r["outT"].T) for r in res.results], axis=0)
    return out.astype(np.float32)
```
